# Optimizing a Trainium2 kernel written in Bass

```python
import math
import jax, jax.numpy as jnp
from jax import lax
import numpy as np

D_MODEL = 1024
BATCH = 4
SEQ = 4096
DEPTH = 2

GRID_W = 64
CTX_LEN = 256
N_MIXERS = 2
N_HEADS = 8
N_KV_HEADS = 2
HEAD_DIM = D_MODEL // N_HEADS
KV_REP = N_HEADS // N_KV_HEADS
QKV_DIM = (N_HEADS + 2 * N_KV_HEADS) * HEAD_DIM
Q_BLOCK = 128
WINDOW = 128
ROPE_THETA = 10000.0
RMS_EPS = 1e-6
NEG_INF = -1e30
N_WINDOW_LAYERS = DEPTH // N_MIXERS
PEER_HEADS = 8
PEER_TOPK = 16
N_KEYS = 128
N_EXPERTS = N_KEYS * N_KEYS
PEER_QDIM = 256
PEER_BLOCK = 128
N_MOD = 6

kernel_name = "hybrid_dit_gqa_window_peer"


def rms_norm(x, gain):
    xf = x.astype(jnp.float32)
    y = xf * lax.rsqrt(jnp.mean(xf * xf, axis=-1, keepdims=True) + RMS_EPS)
    return (y * gain.astype(jnp.float32)).astype(x.dtype)


def axial_rope_tables(n_tokens, dtype):
    rows = n_tokens // GRID_W
    row = jnp.repeat(jnp.arange(rows), GRID_W).astype(jnp.float32)
    col = jnp.tile(jnp.arange(GRID_W), rows).astype(jnp.float32)
    half = HEAD_DIM // 2
    inv = ROPE_THETA ** (-jnp.arange(0, half, 2, dtype=jnp.float32) / half)
    ang = jnp.concatenate([row[:, None] * inv, col[:, None] * inv], axis=-1)
    return jnp.cos(ang).astype(dtype), jnp.sin(ang).astype(dtype)


def apply_axial_rope(x, cos, sin):
    n = x.shape[1]
    xr = x.reshape(x.shape[:-1] + (2, 2, HEAD_DIM // 4))
    x1 = xr[..., 0, :]
    x2 = xr[..., 1, :]
    c = cos.reshape(n, 1, 2, HEAD_DIM // 4)
    s = sin.reshape(n, 1, 2, HEAD_DIM // 4)
    out = jnp.stack([x1 * c - x2 * s, x1 * s + x2 * c], axis=-2)
    return out.reshape(x.shape)


def modulate(h, shift, scale):
    return h * (1 + scale) + shift


def project_qkv(h, w_qkv, q_gain, k_gain, rope):
    b, n, _ = h.shape
    y = h @ w_qkv
    q = y[..., : N_HEADS * HEAD_DIM].reshape(b, n, N_HEADS, HEAD_DIM)
    k = y[..., N_HEADS * HEAD_DIM:(N_HEADS + N_KV_HEADS) * HEAD_DIM].reshape(b, n, N_KV_HEADS, HEAD_DIM)
    v = y[..., (N_HEADS + N_KV_HEADS) * HEAD_DIM:].reshape(b, n, N_KV_HEADS, HEAD_DIM)
    q = rms_norm(q, q_gain)
    k = rms_norm(k, k_gain)
    if rope is not None:
        cos, sin = rope
        q = apply_axial_rope(q, cos, sin)
        k = apply_axial_rope(k, cos, sin)
    q = q.reshape(b, n, N_KV_HEADS, KV_REP, HEAD_DIM)
    return q, k, v


def latent_global_attention(q, k, v, kc, vc):
    b, n = q.shape[:2]
    nblk = n // Q_BLOCK
    scale = HEAD_DIM ** -0.5
    ka = jnp.concatenate([kc, k], axis=1)
    va = jnp.concatenate([vc, v], axis=1)
    qb = jnp.moveaxis(q.reshape(b, nblk, Q_BLOCK, N_KV_HEADS, KV_REP, HEAD_DIM), 1, 0)

    def attend_block(qblk):
        s = jnp.einsum('bqgrd,bkgd->bgrqk', qblk, ka).astype(jnp.float32) * scale
        p = jax.nn.softmax(s, axis=-1)
        return jnp.einsum('bgrqk,bkgd->bqgrd', p.astype(va.dtype), va)

    o = lax.map(attend_block, qb)
    return jnp.moveaxis(o, 0, 1).reshape(b, n, N_HEADS * HEAD_DIM)


def latent_window_attention(q, k, v, kc, vc, sink):
    b, n = q.shape[:2]
    nblk = n // Q_BLOCK
    span = Q_BLOCK + 2 * WINDOW
    scale = HEAD_DIM ** -0.5
    kp = jnp.pad(k, ((0, 0), (WINDOW, WINDOW), (0, 0), (0, 0)))
    vp = jnp.pad(v, ((0, 0), (WINDOW, WINDOW), (0, 0), (0, 0)))
    idx = jnp.arange(nblk)[:, None] * Q_BLOCK + jnp.arange(span)[None, :]
    kb = kp[:, idx]
    vb = vp[:, idx]
    kpos = idx - WINDOW
    qpos = jnp.arange(nblk)[:, None] * Q_BLOCK + jnp.arange(Q_BLOCK)[None, :]
    mask = ((kpos[:, None, :] >= 0) & (kpos[:, None, :] < n)
            & (jnp.abs(qpos[:, :, None] - kpos[:, None, :]) <= WINDOW))
    qb = q.reshape(b, nblk, Q_BLOCK, N_KV_HEADS, KV_REP, HEAD_DIM)
    s_loc = jnp.einsum('bnqgrd,bnkgd->bngrqk', qb, kb).astype(jnp.float32) * scale
    s_loc = jnp.where(mask[None, :, None, None], s_loc, NEG_INF)
    s_ctx = jnp.einsum('bnqgrd,bcgd->bngrqc', qb, kc).astype(jnp.float32) * scale
    sk = jnp.broadcast_to(sink.astype(jnp.float32).reshape(N_KV_HEADS, KV_REP, 1, 1),
                          s_loc.shape[:-1] + (1,))
    p = jax.nn.softmax(jnp.concatenate([s_loc, s_ctx, sk], axis=-1), axis=-1)
    p_loc = p[..., :span]
    p_ctx = p[..., span:span + kc.shape[1]]
    o = (jnp.einsum('bngrqk,bnkgd->bnqgrd', p_loc.astype(vb.dtype), vb)
         + jnp.einsum('bngrqc,bcgd->bnqgrd', p_ctx.astype(vc.dtype), vc))
    return o.reshape(b, n, N_HEADS * HEAD_DIM)


def context_attention(qc, kc, vc, sink):
    b, n = qc.shape[:2]
    scale = HEAD_DIM ** -0.5
    s = jnp.einsum('bqgrd,bkgd->bgrqk', qc, kc).astype(jnp.float32) * scale
    if sink is not None:
        sk = jnp.broadcast_to(sink.astype(jnp.float32).reshape(N_KV_HEADS, KV_REP, 1, 1),
                              s.shape[:-1] + (1,))
        p = jax.nn.softmax(jnp.concatenate([s, sk], axis=-1), axis=-1)[..., :n]
    else:
        p = jax.nn.softmax(s, axis=-1)
    o = jnp.einsum('bgrqk,bkgd->bqgrd', p.astype(vc.dtype), vc)
    return o.reshape(b, n, N_HEADS * HEAD_DIM)


def peer_block(hb, w_q, sub_keys, u_tab, v_tab):
    n = hb.shape[0]
    q = (hb @ w_q).reshape(n, PEER_HEADS, PEER_QDIM)
    q1 = q[..., : PEER_QDIM // 2]
    q2 = q[..., PEER_QDIM // 2:]
    s1 = jnp.einsum('nhd,kd->nhk', q1, sub_keys[0]).astype(jnp.float32)
    s2 = jnp.einsum('nhd,kd->nhk', q2, sub_keys[1]).astype(jnp.float32)
    v1, i1 = lax.top_k(s1, PEER_TOPK)
    v2, i2 = lax.top_k(s2, PEER_TOPK)
    cand_s = (v1[..., :, None] + v2[..., None, :]).reshape(n, PEER_HEADS, PEER_TOPK * PEER_TOPK)
    cand_i = (i1[..., :, None] * N_KEYS + i2[..., None, :]).reshape(n, PEER_HEADS, PEER_TOPK * PEER_TOPK)
    top_s, pos = lax.top_k(cand_s, PEER_TOPK)
    expert = jnp.take_along_axis(cand_i, pos, axis=-1)
    g = jax.nn.softmax(top_s, axis=-1)
    u = u_tab[expert]
    act = jax.nn.gelu(jnp.einsum('nhkd,nd->nhk', u, hb).astype(jnp.float32))
    w = (g * act).astype(hb.dtype)
    return jnp.einsum('nhk,nhkd->nd', w, v_tab[expert])


def peer_ffn(h, w_q, sub_keys, u_tab, v_tab):
    b, n, d = h.shape
    hb = h.reshape(-1, PEER_BLOCK, d)
    out = lax.map(lambda blk: peer_block(blk, w_q, sub_keys, u_tab, v_tab), hb)
    return out.reshape(b, n, d)


def setup_inputs(seed: int = 0) -> dict:
    key = jax.random.key(seed)
    ks = jax.random.split(key, 18)
    f32 = jnp.float32
    x = jax.random.normal(ks[0], (BATCH, SEQ, D_MODEL), f32)
    c = jax.random.normal(ks[1], (BATCH, D_MODEL), f32)
    ctx = jax.random.normal(ks[2], (BATCH, CTX_LEN, D_MODEL), f32)
    c_ctx = jax.random.normal(ks[3], (D_MODEL,), f32)
    ada_w = jax.random.normal(ks[4], (DEPTH, D_MODEL, N_MOD * D_MODEL), f32) * (0.5 * D_MODEL ** -0.5)
    ada_b = jax.random.normal(ks[5], (DEPTH, N_MOD * D_MODEL), f32) * 0.1
    norm1_gain = 1.0 + 0.02 * jax.random.normal(ks[6], (DEPTH, D_MODEL), f32)
    norm2_gain = 1.0 + 0.02 * jax.random.normal(ks[7], (DEPTH, D_MODEL), f32)
    w_qkv = jax.random.normal(ks[8], (DEPTH, D_MODEL, QKV_DIM), f32) * D_MODEL ** -0.5
    q_norm_gain = 1.0 + 0.02 * jax.random.normal(ks[9], (DEPTH, HEAD_DIM), f32)
    k_norm_gain = 1.0 + 0.02 * jax.random.normal(ks[10], (DEPTH, HEAD_DIM), f32)
    w_o = jax.random.normal(ks[11], (DEPTH, N_HEADS * HEAD_DIM, D_MODEL), f32) * (N_HEADS * HEAD_DIM) ** -0.5
    attn_sinks = jax.random.normal(ks[12], (N_WINDOW_LAYERS, N_HEADS), f32) * 0.5
    peer_w_q = jax.random.normal(ks[13], (DEPTH, D_MODEL, PEER_HEADS * PEER_QDIM), f32) * D_MODEL ** -0.5
    peer_sub_keys = jax.random.normal(ks[14], (DEPTH, 2, N_KEYS, PEER_QDIM // 2), f32) * (PEER_QDIM // 2) ** -0.5
    peer_u = jax.random.normal(ks[15], (DEPTH, N_EXPERTS, D_MODEL), f32) * D_MODEL ** -0.5
    peer_v = jax.random.normal(ks[16], (DEPTH, N_EXPERTS, D_MODEL), f32) * PEER_HEADS ** -0.5
    return {"x": x, "c": c, "ctx": ctx, "c_ctx": c_ctx, "ada_w": ada_w, "ada_b": ada_b,
            "norm1_gain": norm1_gain, "norm2_gain": norm2_gain, "w_qkv": w_qkv,
            "q_norm_gain": q_norm_gain, "k_norm_gain": k_norm_gain, "w_o": w_o,
            "attn_sinks": attn_sinks, "peer_w_q": peer_w_q, "peer_sub_keys": peer_sub_keys,
            "peer_u": peer_u, "peer_v": peer_v}


def reference(x, c, ctx, c_ctx, ada_w, ada_b, norm1_gain, norm2_gain, w_qkv, q_norm_gain,
              k_norm_gain, w_o, attn_sinks, peer_w_q, peer_sub_keys, peer_u, peer_v):
    n_lat = x.shape[1]
    rope = axial_rope_tables(n_lat, x.dtype)
    xc = ctx
    for i in range(DEPTH):
        last = i == DEPTH - 1
        mod = jax.nn.silu(c) @ ada_w[i] + ada_b[i]
        sh1, sc1, g1, sh2, sc2, g2 = [m[:, None, :] for m in jnp.split(mod, N_MOD, axis=-1)]
        modc = jax.nn.silu(c_ctx) @ ada_w[i] + ada_b[i]
        csh1, csc1, cg1, csh2, csc2, cg2 = jnp.split(modc, N_MOD, axis=-1)

        h = modulate(rms_norm(x, norm1_gain[i]), sh1, sc1)
        hc = modulate(rms_norm(xc, norm1_gain[i]), csh1, csc1)
        q, k, v = project_qkv(h, w_qkv[i], q_norm_gain[i], k_norm_gain[i], rope)
        qc, kc, vc = project_qkv(hc, w_qkv[i], q_norm_gain[i], k_norm_gain[i], None)
        if i % N_MIXERS == 0:
            o = latent_global_attention(q, k, v, kc, vc)
            sink = None
        else:
            sink = attn_sinks[i // N_MIXERS]
            o = latent_window_attention(q, k, v, kc, vc, sink)
        x = x + g1 * (o @ w_o[i])
        if not last:
            oc = context_attention(qc, kc, vc, sink)
            xc = xc + cg1 * (oc @ w_o[i])

        h2 = modulate(rms_norm(x, norm2_gain[i]), sh2, sc2)
        x = x + g2 * peer_ffn(h2, peer_w_q[i], peer_sub_keys[i], peer_u[i], peer_v[i])
        if not last:
            h2c = modulate(rms_norm(xc, norm2_gain[i]), csh2, csc2)
            xc = xc + cg2 * peer_ffn(h2c, peer_w_q[i], peer_sub_keys[i], peer_u[i], peer_v[i])
    return x
```

```python
import os
import numpy as np
import concourse.bass as bass
import concourse.mybir as mybir
from concourse.bass_utils import run_bass_kernel_spmd
from contextlib import ExitStack

F32 = mybir.dt.float32
BF16 = mybir.dt.bfloat16
U32 = mybir.dt.uint32
ALU = mybir.AluOpType
AF = mybir.ActivationFunctionType
AX = mybir.AxisListType

D = 1024
NT_IN = 34
NT_ST = 19
EPS = 1e-6
SCALE = 128.0 ** -0.5


class Sched:
    ENG = ('pe', 'act', 'dve', 'pool', 'sp')

    def __init__(self, nc, es):
        self.nc = nc
        self.es = es
        self.ops = {e: [] for e in self.ENG}
        self.lastw = {}
        self.readers = {}
        self.esem = {e: es.enter_context(nc.semaphore('s_' + e)) for e in self.ENG}
        self.dsem = {}

    def new_dsem(self, name):
        self.dsem[name] = [self.es.enter_context(self.nc.semaphore('d_' + name)), 0]
        return name

    def _deps(self, reads, writes):
        toks = []
        for k in reads:
            if k in self.lastw:
                toks.append(self.lastw[k])
        for k in writes:
            if k in self.lastw:
                toks.append(self.lastw[k])
            toks.extend(self.readers.get(k, {}).values())
        return toks

    def _commit(self, tok, reads, writes):
        for k in reads:
            self.readers.setdefault(k, {})[tok[1]] = tok
        for k in writes:
            self.lastw[k] = tok
            self.readers[k] = {}

    def op(self, eng, fn, reads=(), writes=()):
        toks = self._deps(reads, writes)
        tok = ('E', eng, len(self.ops[eng]))
        self.ops[eng].append(dict(fn=fn, deps=toks, ms=False, dsem=None))
        self._commit(tok, reads, writes)
        return tok

    def dma(self, eng, fns, sem, reads=(), writes=()):
        toks = self._deps(reads, writes)
        self.dsem[sem][1] += 16 * len(fns)
        tok = ('D', sem, self.dsem[sem][1])
        self.ops[eng].append(dict(fn=fns, deps=toks, ms=False, dsem=sem))
        self._commit(tok, reads, writes)
        return tok

    def wait_all(self, eng, toks):
        self.ops[eng].append(dict(fn=None, deps=list(toks), ms=False, dsem=None))

    def barrier(self):
        toks = []
        for e in self.ENG:
            for i in range(len(self.ops[e]) - 1, -1, -1):
                o = self.ops[e][i]
                if o['fn'] is not None and o['dsem'] is None:
                    toks.append(('E', e, i))
                    break
        for name, (h, cnt) in self.dsem.items():
            if cnt > 0:
                toks.append(('D', name, cnt))
        for e in self.ENG:
            self.wait_all(e, toks)
        self.lastw = {}
        self.readers = {}

    def emit(self):
        nc = self.nc
        for e in self.ENG:
            for o in self.ops[e]:
                nd = []
                for t in o['deps']:
                    if t[0] == 'E':
                        if t[1] == e and e == 'pe':
                            continue
                        self.ops[t[1]][t[2]]['ms'] = True
                    nd.append(t)
                o['deps'] = nd
        msval = {}
        for e in self.ENG:
            c = 0
            for i, o in enumerate(self.ops[e]):
                if o['ms']:
                    c += 1
                msval[(e, i)] = c

        def run(e, engobj):
            known = {}
            for i, o in enumerate(self.ops[e]):
                for t in o['deps']:
                    if t[0] == 'E':
                        s, v, h = t[1], msval[(t[1], t[2])], self.esem[t[1]]
                    else:
                        s, v, h = t[1], t[2], self.dsem[t[1]][0]
                    if known.get(s, 0) >= v:
                        continue
                    known[s] = v
                    engobj.wait_ge(h, v)
                if o['fn'] is None:
                    continue
                if o['dsem'] is not None:
                    for f in o['fn']:
                        f(engobj).then_inc(self.dsem[o['dsem']][0], 16)
                else:
                    ins = o['fn'](engobj)
                    if o['ms']:
                        ins.then_inc(self.esem[e], 1)

        with nc.Block() as block:
            @block.tensor
            def _(t):
                run('pe', t)

            @block.scalar
            def _(t):
                run('act', t)

            @block.vector
            def _(t):
                run('dve', t)

            @block.gpsimd
            def _(t):
                run('pool', t)

            @block.sync
            def _(t):
                run('sp', t)


def build(dbg=None):
    nc = bass.Bass("TRN2", target_bir_lowering=False)

    def din(name, shape):
        return nc.dram_tensor(name, shape, F32, kind="ExternalInput").ap()

    xin = din("xin", [NT_IN * 128, D])
    cosd = din("cosd", [NT_IN * 128, 64])
    sind = din("sind", [NT_IN * 128, 64])
    maskd = din("maskd", [128, 4, 512])
    cTd = din("cT", [128, 8, 2])
    ada_w = din("ada_w", [2, D, 6 * D])
    adabT = din("adabT", [2, 128, 48])
    ada_b = din("ada_b", [2, 6 * D])
    n1T = din("n1T", [2, 128, 8])
    n2T = din("n2T", [2, 128, 8])
    qkg = din("qkg", [2, 1280])
    w_qkv = din("w_qkv", [2, D, 1536])
    w_o = din("w_o", [2, D, D])
    w_q = din("peer_w_q", [2, D, 2048])
    ksT = din("ksT", [2, 128, 2, 128])
    uT = din("uT", [2, 128, 128, 8, 128])
    vT = din("vT", [2, 128, 128, D])
    sinks = din("sinks", [1, 8])
    out = nc.dram_tensor("out", [2048, D], F32, kind="ExternalOutput").ap()
    xs = nc.dram_tensor("xs", [NT_ST * 128, D], F32, kind="Internal").ap()
    dbg_out = None
    if dbg is not None:
        dbg_out = nc.dram_tensor("dbg", [128, 1024], F32, kind="ExternalOutput").ap()

    with ExitStack() as es:
        S = Sched(nc, es)
        ARENA = 206 * 1024 // 2
        arena = es.enter_context(nc.sbuf_tensor("arena", [128, ARENA], BF16))
        banks = [es.enter_context(nc.psum_tensor("pb%d" % i, [128, 512], F32)) for i in range(8)]
        pk = ["ps%d" % i for i in range(8)]
        st = dict(off=0)

        def alloc(shape, dt):
            shape = list(shape[1:])
            n = int(np.prod(shape))
            units = n * (2 if dt in (F32, U32) else 1)
            a = st['off']
            st['off'] = a + units + (units % 2)
            assert st['off'] <= ARENA, ("arena overflow", st['off'] * 2)
            v = arena[:, a:a + units]
            if dt != BF16:
                v = v.bitcast(dt)
            if len(shape) > 1:
                names = "abcd"[:len(shape)]
                kw = {names[i]: shape[i] for i in range(len(shape))}
                v = v.rearrange("p (%s) -> p %s" % (" ".join(names), " ".join(names)), **kw)
            return v

        def MM(o, lhsT, rhs, start, stop, reads, writes):
            S.op('pe', lambda e: e.matmul(o, lhsT=lhsT, rhs=rhs, start=start, stop=stop), reads, writes)

        def TR(o, in_, idn, reads, writes):
            S.op('pe', lambda e: e.transpose(o, in_, idn), reads, writes)

        def ACTV(o, in_, func, reads, writes, scale=None, bias=None, eng='act'):
            kw = {}
            if scale is not None:
                kw['scale'] = scale
            if bias is not None:
                kw['bias'] = bias
            S.op(eng, lambda e: e.activation(out=o, in_=in_, func=func, **kw), reads, writes)

        def TT(eng, o, a, b, op, reads, writes):
            S.op(eng, lambda e: e.tensor_tensor(out=o, in0=a, in1=b, op=op), reads, writes)

        def TS(eng, o, a, s1, s2, op0, op1, reads, writes):
            if s2 is None:
                S.op(eng, lambda e: e.tensor_single_scalar(out=o, in_=a, scalar=s1, op=op0), reads, writes)
            else:
                S.op(eng, lambda e: e.tensor_scalar(out=o, in0=a, scalar1=s1, scalar2=s2, op0=op0, op1=op1), reads, writes)

        def STT(eng, o, a, sc, b, op0, op1, reads, writes):
            S.op(eng, lambda e: e.scalar_tensor_tensor(out=o, in0=a, scalar=sc, in1=b, op0=op0, op1=op1), reads, writes)

        def CP(eng, o, a, reads, writes):
            if eng == 'act':
                S.op(eng, lambda e: e.activation(out=o, in_=a, func=AF.Identity), reads, writes)
            else:
                S.op(eng, lambda e: e.tensor_copy(out=o, in_=a), reads, writes)

        def RSUM(eng, o, a, reads, writes):
            S.op(eng, lambda e: e.tensor_reduce(out=o, in_=a, axis=AX.X, op=ALU.add), reads, writes)

        def MEMSET(eng, o, val, writes):
            S.op(eng, lambda e: e.memset(o, val), (), writes)

        mctr = [0]

        def DMA(eng, o, i, sem, reads, writes):
            if sem == "misc":
                sem = "misc%d" % (mctr[0] % 12)
                mctr[0] += 1
            return S.dma(eng, [lambda e: e.dma_start(out=o, in_=i)], sem, reads, writes)

        def psbf(b):
            return banks[b][:].bitcast(BF16)

        ident = alloc([128, 128], BF16)
        identf = alloc([128, 128], F32)
        iota_c = alloc([128, 128], F32)
        iota16 = alloc([128, 16], F32)
        iota_cb = alloc([128, 128], BF16)
        esink = alloc([128, 8], F32)
        scT = alloc([128, 8, 2], F32)
        modT = alloc([128, 6, 8, 2], F32)
        AB = alloc([128, 2, 2, 2, 8], F32)
        gates = [[alloc([128, D], F32) for j in range(2)] for m in range(2)]
        nT = alloc([128, 2, 8], F32)
        abT = alloc([128, 48], F32)
        epsb = alloc([128, 1], F32)
        PBASE = st['off']

        for n_ in ["ld_a", "ld_b", "ld_c", "ld_d", "wt", "xl0", "xl1", "rp0", "rp1", "xst", "tu0", "tu1", "tv0", "tv1", "tu2", "tv2",
                   "xr0", "xr1", "wt2"] + ["misc%d" % i for i in range(12)]:
            S.new_dsem(n_)

        S.op('pool', lambda e: e.iota(identf, [[1, 128]], base=0, channel_multiplier=-1,
                                      allow_small_or_imprecise_dtypes=True), (), ["identf"])
        TS('dve', identf, identf, 0.0, None, ALU.is_equal, None, ["identf"], ["identf"])
        CP('dve', ident, identf, ["identf"], ["ident"])
        S.op('pool', lambda e: e.iota(iota_c, [[1, 128]], base=0, channel_multiplier=0,
                                      allow_small_or_imprecise_dtypes=True), (), ["iota_c"])
        S.op('pool', lambda e: e.iota(iota16, [[1, 16]], base=0, channel_multiplier=0,
                                      allow_small_or_imprecise_dtypes=True), (), ["iota16"])
        MEMSET('dve', epsb, EPS, ["epsb"])
        CP('dve', iota_cb, iota_c, ["iota_c"], ["iota_cb"])
        DMA('sp', scT, cTd, "misc", [], ["scT"])
        DMA('sp', esink, sinks.partition_broadcast(128), "misc", [], ["esink"])
        ACTV(esink, esink, AF.Exp, ["esink"], ["esink"])
        ACTV(scT, scT, AF.Silu, ["scT"], ["scT"])

        def mod_phase(l):
            st['off'] = PBASE
            crep = [alloc([128, 8, 128], F32) for j in range(2)]
            piece = [alloc([128, 8, D], F32) for i in range(2)]
            bcb = alloc([128, D], F32)
            for j in range(2):
                CP('dve', crep[j], scT[:, :, j:j + 1].to_broadcast([128, 8, 128]), ["scT"], ["crep%d" % j])
            DMA('sp', abT, adabT[l], "misc", [], ["abT"])
            DMA('sp', nT[:, 0, :], n1T[l], "misc", [], ["nT"])
            DMA('sp', nT[:, 1, :], n2T[l], "misc", [], ["nT"])
            semn = ["ld_a", "ld_b"]
            for m in range(6):
                pc = piece[m % 2]
                pkey = "piece%d" % (m % 2)
                DMA('sp', pc, ada_w[l, :, m * D:(m + 1) * D].rearrange("(kc p) f -> p kc f", p=128), semn[m % 2], [], [pkey])
                if m in (0, 1, 3, 4):
                    pb = banks[m % 2]
                    pv = pb[:, 0:16].rearrange("p (f j) -> p f j", j=2)
                    for fc in range(8):
                        for kc in range(8):
                            MM(pv[:, fc, :], pc[:, kc, fc * 128:(fc + 1) * 128], scT[:, kc, :], kc == 0, kc == 7,
                               [pkey, "scT"], [pk[m % 2]])
                    TT('dve', modT[:, m], pv, abT[:, m * 8:(m + 1) * 8].unsqueeze(2).to_broadcast([128, 8, 2]), ALU.add,
                       [pk[m % 2], "abT"], ["modT%d" % m])
                else:
                    gi = 0 if m == 2 else 1
                    DMA('sp', bcb, ada_b[l:l + 1, m * D:(m + 1) * D].partition_broadcast(128), "ld_c", [], ["bcb"])
                    for j in range(2):
                        for dh in range(2):
                            b_ = 2 + (j * 2 + dh) % 2
                            for kc in range(8):
                                MM(banks[b_][:], crep[j][:, kc, :], pc[:, kc, dh * 512:(dh + 1) * 512], kc == 0, kc == 7,
                                   [pkey, "crep%d" % j], [pk[b_]])
                            TT('dve', gates[gi][j][:, dh * 512:(dh + 1) * 512], banks[b_][:], bcb[:, dh * 512:(dh + 1) * 512],
                               ALU.add, [pk[b_], "bcb"], ["gate%d%d" % (gi, j)])
            for ni, (msh, msc) in enumerate([(0, 1), (3, 4)]):
                for j in range(2):
                    TS('dve', AB[:, ni, 0, j, :], modT[:, msc, :, j], 1.0, None, ALU.add, None, ["modT%d" % msc], ["AB"])
                    TT('dve', AB[:, ni, 0, j, :], AB[:, ni, 0, j, :], nT[:, ni, :], ALU.mult, ["AB", "nT"], ["AB"])
                    CP('dve', AB[:, ni, 1, j, :], modT[:, msh, :, j], ["modT%d" % msh], ["AB"])
            S.barrier()

        def norm_mod(*a, **k):
            for _ in norm_mod_g(*a, **k):
                pass

        def norm_mod_g(xt, xkey, ni, j, hT_view, hkey, sq, xn, small, bank, sqkey="sq", smallkey="small", evac_act=False):
            ACTV(sq, xt, AF.Square, [xkey], [sqkey])
            RSUM('dve', small[:, 0:1], sq, [sqkey], [smallkey])
            yield
            ACTV(small[:, 1:2], small[:, 0:1], AF.Sqrt, [smallkey, "epsb"], [smallkey], scale=1.0 / D, bias=epsb)
            S.op('dve', lambda e: e.reciprocal(small[:, 2:3], small[:, 1:2]), [smallkey], [smallkey])
            yield
            ACTV(xn, xt, AF.Identity, [xkey, smallkey], ["xn"], scale=small[:, 2:3])
            pv = psbf(bank).rearrange("p (k n) -> p k n", n=128)
            for kc in range(8):
                TR(pv[:, kc, :], xn[:, kc * 128:(kc + 1) * 128], ident, ["xn", "ident"], [pk[bank]])
            yield
            for kc in range(8):
                if evac_act:
                    ACTV(hT_view[:, kc, :], pv[:, kc, :], AF.Identity, [pk[bank], "AB"], [hkey],
                         scale=AB[:, ni, 0, j, kc:kc + 1], bias=AB[:, ni, 1, j, kc:kc + 1])
                else:
                    TS('dve', hT_view[:, kc, :], pv[:, kc, :], AB[:, ni, 0, j, kc:kc + 1], AB[:, ni, 1, j, kc:kc + 1],
                       ALU.mult, ALU.add, [pk[bank], "AB"], [hkey])

        def attn_phase(l):
            st['off'] = PBASE
            kT = alloc([128, 2, NT_IN, 128], BF16)
            Vst = alloc([128, NT_IN, 2, 130], BF16)
            qT = alloc([128, 8, NT_ST, 128], BF16)
            PT = [alloc([128, 512], BF16) for i in range(2)]
            wqkv = alloc([128, 8, 1536], BF16)
            wo = alloc([128, 8, D], BF16)
            xt = [alloc([128, D], F32) for i in range(2)]
            xn = alloc([128, D], BF16)
            hT = [alloc([128, 8, 128], BF16) for i in range(2)]
            ybuf = [alloc([128, 10, 128], F32) for i in range(2)]
            tmpAB = [alloc([128, 1280], F32) for i in range(2)]
            tmpA = [tt_[:, 0:640].rearrange("p (h a f) -> p h a f", a=2, f=32) for tt_ in tmpAB]
            tmpB = [tt_[:, 640:1280].rearrange("p (h a f) -> p h a f", a=2, f=32) for tt_ in tmpAB]
            qkbuf = [alloc([128, 10, 128], BF16) for i in range(2)]
            smallb = [alloc([128, 32], F32) for i in range(2)]
            cs = [[alloc([128, 2, 32], F32) for k in range(2)] for i in range(2)]
            small = alloc([128, 32], F32)
            ob = alloc([128, 8, 128], BF16)
            oT = alloc([128, 8, 128], BF16)
            rtmp = alloc([128, D], F32)
            den = alloc([128, 8], F32)
            masks = alloc([128, 4, 512], F32)
            qkgb = alloc([128, 10, 128], F32)
            DMA('sp', masks, maskd, "misc", [], ["masks"])
            DMA('sp', qkgb.rearrange("p a b -> p (a b)"), qkg[l:l + 1, :].partition_broadcast(128), "misc", [], ["qkgb"])

            DMA('pool', wqkv, w_qkv[l].rearrange("(kc p) f -> p kc f", p=128), "wt", [], ["wqkv"])
            DMA('pool', wo, w_o[l].rearrange("(kc p) f -> p kc f", p=128), "wt2", [], ["wo"])
            MEMSET('pool', Vst[:, :, :, 128:130], 1.0, ["Vst"])

            if l == 0:
                tiles = [(t, xin, t, (t if t <= 16 else (t - 15 if t >= 32 else None))) for t in range(NT_IN)]
            else:
                tiles = [(si, xs, si, (si if si <= 15 else None)) for si in range(17)] + \
                        [(32, xs, 17, None), (33, xs, 18, None)]
            SMALL = int(os.environ.get("K_SMALL", "0"))
            CUT = int(os.environ.get("K_CUT", "99"))
            if SMALL:
                keep = {0, 1, 15, 16, 17, 32, 33}
                tiles = [tt for tt in tiles if tt[0] in keep]

            def load_tile(i):
                t, src, stile, qi = tiles[i]
                b = i % 2
                DMA('sp', xt[b], src[stile * 128:(stile + 1) * 128, :], "xl%d" % b, [], ["xt%d" % b])

            def load_rope(i):
                t, src, stile, qi = tiles[i]
                b = i % 2
                ri = t
                S.dma('sp', [lambda e: e.dma_start(out=cs[b][0].rearrange("p a b -> p (a b)"), in_=cosd[ri * 128:(ri + 1) * 128, :]),
                             lambda e: e.dma_start(out=cs[b][1].rearrange("p a b -> p (a b)"), in_=sind[ri * 128:(ri + 1) * 128, :])],
                      "rp%d" % b, [], ["cs%d" % b])

            load_tile(0)
            load_rope(0)

            def stage1(i):
                t, src, stile, qi = tiles[i]
                b = i % 2
                if i + 1 < len(tiles):
                    load_tile(i + 1)
                j = 1 if t >= 32 else 0
                y, ysq, qk, small = ybuf[b], tmpAB[b].rearrange("p (h d) -> p h d", d=128), qkbuf[b], smallb[b]
                tmp = [tmpA[b], tmpB[b]]
                KY, KQ, KS, KT0, KT1 = "y%d" % b, "qk%d" % b, "small%d" % b, "tmpA%d" % b, "tmpB%d" % b
                yield from norm_mod_g(xt[b], "xt%d" % b, 0, j, hT[b], "hT%d" % b, rtmp, xn, small, 3, sqkey="rtmp", smallkey="smalln%d" % b, evac_act=True)

            def stage1b(i):
                t, src, stile, qi = tiles[i]
                b = i % 2
                y = ybuf[b]
                KY = "y%d" % b
                cbs = [0, 1, 2] if qi is not None else [2]
                for cb in cbs:
                    for kc in range(8):
                        MM(banks[cb][:], hT[b][:, kc, :], wqkv[:, kc, cb * 512:(cb + 1) * 512], kc == 0, kc == 7,
                           ["hT%d" % b, "wqkv"], [pk[cb]])
                nh = 10 if qi is not None else 2
                h0 = 0 if qi is not None else 8
                yv = y[:, h0:10, :]
                yield
                SUB = os.environ.get("K_SUB", "abc")
                if "b" not in SUB:
                    return
                if qi is not None:
                    ACTV(y[:, 0:4, :], banks[0][:].rearrange("p (h d) -> p h d", d=128), AF.Identity, [pk[0]], [KY])
                    ACTV(y[:, 4:8, :], banks[1][:].rearrange("p (h d) -> p h d", d=128), AF.Identity, [pk[1]], [KY])
                ACTV(y[:, 8:10, :], banks[2][:, 0:256].rearrange("p (h d) -> p h d", d=128), AF.Identity, [pk[2]], [KY])
                if "c" not in SUB:
                    return
                CP('act', Vst[:, t, :, 0:128], banks[2][:, 256:512].rearrange("p (g d) -> p g d", d=128), [pk[2]], ["Vst"])

            def stage2(i):
                t, src, stile, qi = tiles[i]
                b = i % 2
                if i + 1 < len(tiles):
                    load_rope(i + 1)
                j = 1 if t >= 32 else 0
                y, ysq, qk, small = ybuf[b], tmpAB[b].rearrange("p (h d) -> p h d", d=128), qkbuf[b], smallb[b]
                tmp = [tmpA[b], tmpB[b]]
                KY, KQ, KS, KT0, KT1 = "y%d" % b, "qk%d" % b, "small%d" % b, "tmpA%d" % b, "tmpB%d" % b
                nh = 10 if qi is not None else 2
                h0 = 0 if qi is not None else 8
                yv = y[:, h0:10, :]
                if CUT < 3:
                    return
                ACTV(ysq[:, h0:10, :], yv, AF.Square, [KY], [KT0, KT1])
                RSUM('dve', small[:, 8 + h0:18], ysq[:, h0:10, :], [KT0, KT1], [KS])
                yield
                ACTV(small[:, 8 + h0:18], small[:, 8 + h0:18], AF.Sqrt, [KS, "epsb"], [KS], scale=1.0 / 128, bias=epsb)
                yield
                S.op('dve', lambda e, h0=h0, small=small: e.reciprocal(small[:, 8 + h0:18], small[:, 8 + h0:18]), [KS], [KS])
                TT('dve', yv, yv, small[:, 8 + h0:18].unsqueeze(2).to_broadcast([128, nh, 128]), ALU.mult, [KY, KS], [KY])
                TT('dve', yv, yv, qkgb[:, h0:10, :], ALU.mult, [KY, "qkgb"], [KY])
                if CUT < 4:
                    return
                y4 = y.rearrange("p h (a t f) -> p h a t f", a=2, t=2)
                q4 = qk.rearrange("p h (a t f) -> p h a t f", a=2, t=2)
                x1 = y4[:, h0:10, :, 0, :]
                x2 = y4[:, h0:10, :, 1, :]
                cb_ = cs[b][0].unsqueeze(1).to_broadcast([128, nh, 2, 32])
                sb_ = cs[b][1].unsqueeze(1).to_broadcast([128, nh, 2, 32])
                t0 = tmp[0][:, h0:10]
                t1 = tmp[1][:, h0:10]
                ck = "cs%d" % b
                TT('dve', t0, x1, cb_, ALU.mult, [KY, ck], [KT0])
                TT('dve', t1, x2, sb_, ALU.mult, [KY, ck], [KT1])
                TT('dve', q4[:, h0:10, :, 0, :], t0, t1, ALU.subtract, [KT0, KT1], [KQ])
                TT('dve', t0, x1, sb_, ALU.mult, [KY, ck], [KT0])
                TT('dve', t1, x2, cb_, ALU.mult, [KY, ck], [KT1])
                TT('dve', q4[:, h0:10, :, 1, :], t0, t1, ALU.add, [KT0, KT1], [KQ])
                yield
                if CUT < 5:
                    return
                if qi is not None:
                    pv = psbf(4).rearrange("p (h n) -> p h n", n=128)
                    for h in range(8):
                        TR(pv[:, h, :], qk[:, h, :], ident, [KQ, "ident"], [pk[4]])
                    CP('act', qT[:, :, qi, :], pv, [pk[4]], ["qT"])
                pv2 = psbf(5).rearrange("p (h n) -> p h n", n=128)
                for g in range(2):
                    TR(pv2[:, g, :], qk[:, 8 + g, :], ident, [KQ, "ident"], [pk[5]])
                yield
                CP('act', kT[:, :, t, :], pv2[:, 0:2, :], [pk[5]], ["kT"])


            def rr(gens):
                gens = [g_ for g_ in gens if g_ is not None]
                while gens:
                    for g_ in list(gens):
                        try:
                            next(g_)
                        except StopIteration:
                            gens.remove(g_)

            rr([stage1(0)])
            if len(tiles) > 1:
                rr([stage1(1), stage1b(0)])
            else:
                rr([stage1b(0)])
            for i in range(len(tiles)):
                rr([stage2(i),
                    stage1b(i + 1) if i + 1 < len(tiles) else None,
                    stage1(i + 2) if i + 2 < len(tiles) else None])

            if l == 0:
                qtiles = list(range(NT_ST))
            else:
                qtiles = list(range(16))

            def load_res(i):
                si = qtiles[i]
                b = i % 2
                if l == 0:
                    stile = si if si <= 16 else si + 15
                    src = xin
                else:
                    stile, src = si, xs
                DMA('sp', xt[b], src[stile * 128:(stile + 1) * 128, :], "xl%d" % b, [], ["xt%d" % b])

            if SMALL:
                qtiles = [qq_ for qq_ in qtiles if qq_ in (0, 15, 17)][:SMALL]
            if CUT < 6:
                qtiles = []
            if qtiles:
                load_res(0)
            pti = 0
            for i, si in enumerate(qtiles):
                b = i % 2
                if i + 1 < len(qtiles):
                    load_res(i + 1)
                isctx = si >= 17
                if l == 0:
                    keys = [(32, None), (33, None)] if isctx else [(t, None) for t in range(NT_IN)]
                else:
                    prev = (si - 1, 0) if si > 0 else (16, 2)
                    nxt = (si + 1, 1) if si < 15 else (16, 3)
                    keys = [prev, (si, None), nxt, (32, None), (33, None)]
                if SMALL:
                    keys = [kk_ for kk_ in keys if kk_[0] in keep]
                for g in range(2):
                    nk = len(keys)
                    base = pti
                    pti += nk

                    def emit_st(ki, g=g, base=base, si=si, keys=keys):
                        t = keys[ki][0]
                        sb_i = 4 + ((base + ki) % 2)
                        MM(banks[sb_i][:].rearrange("p (r q) -> p r q", q=128), kT[:, g, t, :], qT[:, 4 * g:4 * g + 4, si, :], True, True,
                           ["kT", "qT"], [pk[sb_i]])

                    emit_st(0)
                    for ki, (t, mi) in enumerate(keys):
                        sb_i = 4 + ((base + ki) % 2)
                        pt = PT[(base + ki) % 2]
                        ptk = "PT%d" % ((base + ki) % 2)
                        if ki + 1 < nk:
                            emit_st(ki + 1)
                        ACTV(pt, banks[sb_i][:], AF.Exp, [pk[sb_i]], [ptk], scale=SCALE)
                        if mi is not None:
                            TT('dve', pt, pt, masks[:, mi, :], ALU.mult, [ptk, "masks"], [ptk])
                        if CUT < 7:
                            continue
                        for r in range(4):
                            MM(banks[r][:, 0:129], pt[:, r * 128:(r + 1) * 128], Vst[:, t, g, 0:129], ki == 0, ki == nk - 1,
                               [ptk, "Vst"], [pk[r]])
                    if CUT < 8:
                        continue
                    for r in range(4):
                        h = 4 * g + r
                        if l == 1:
                            TT('dve', den[:, h:h + 1], banks[r][:, 128:129], esink[:, h:h + 1], ALU.add, [pk[r], "esink"], ["den"])
                        else:
                            CP('dve', den[:, h:h + 1], banks[r][:, 128:129], [pk[r]], ["den"])
                        S.op('dve', lambda e, h=h: e.reciprocal(den[:, h:h + 1], den[:, h:h + 1]), ["den"], ["den"])
                        ACTV(ob[:, h, :], banks[r][:, 0:128], AF.Identity, [pk[r], "den"], ["ob"], scale=den[:, h:h + 1])
                if CUT < 9:
                    continue
                pv = psbf(6).rearrange("p (h n) -> p h n", n=128)
                for h in range(8):
                    TR(pv[:, h, :], ob[:, h, :], ident, ["ob", "ident"], [pk[6]])
                CP('dve', oT, pv, [pk[6]], ["oT"])
                j = 1 if isctx else 0
                for dh in range(2):
                    for h in range(8):
                        MM(banks[7][:], oT[:, h, :], wo[:, h, dh * 512:(dh + 1) * 512], h == 0, h == 7, ["oT", "wo"], [pk[7]])
                    TT('dve', rtmp[:, dh * 512:(dh + 1) * 512], banks[7][:], gates[0][j][:, dh * 512:(dh + 1) * 512], ALU.mult,
                       [pk[7], "gate0%d" % j], ["rtmp"])
                TT('pool', rtmp, rtmp, xt[b], ALU.add, ["rtmp", "xt%d" % b], ["rtmp"])
                DMA('sp', xs[si * 128:(si + 1) * 128, :], rtmp, "xst", ["rtmp"], [])
            S.barrier()

        def peer_phase(l):
            st['off'] = PBASE
            ntl = NT_ST if l == 0 else 16
            wq = alloc([128, 8, 2048], BF16)
            ks = alloc([128, 2, 128], BF16)
            GT = alloc([128, 128, 256], BF16)
            ub = [alloc([128, 2, 8, 128], BF16) for i in range(3)]
            vb = [alloc([128, 2, D], BF16) for i in range(3)]
            big = alloc([128, 2048], F32)
            wk = alloc([128, 256], F32)
            vtop = alloc([128, 16, 16], F32)
            ix = alloc([128, 16, 16], U32)
            ixf = alloc([128, 16, 16], F32)
            ts = alloc([128, 8, 16], F32)
            pos = alloc([128, 8, 16], U32)
            pa = alloc([128, 8, 16], U32)
            pbq = alloc([128, 8, 16], U32)
            af = alloc([128, 8, 16], F32)
            bf = alloc([128, 8, 16], F32)
            e1f = alloc([128, 8, 16], F32)
            e2f = alloc([128, 8, 16], F32)
            gf = alloc([128, 8, 16], F32)
            zz = alloc([128, 16], F32)
            eT = alloc([128, 3, 256], BF16)
            Aoh2 = [alloc([128, 8, 128], BF16) for i in range(2)]
            Boh2 = [alloc([128, 8, 128], BF16) for i in range(2)]
            h2T = [alloc([128, 8, 256], BF16) for i in range(2)]
            qpT = alloc([128, 16, 256], BF16)
            xt = alloc([128, D], F32)
            xres = alloc([128, D], F32)
            xn = alloc([128, D], BF16)
            small = alloc([128, 32], F32)
            xsb = [alloc([128, 256], F32) for i in range(3)]
            g1_ = [alloc([128, 256], F32) for i in range(3)]
            g2_ = [alloc([128, 256], BF16) for i in range(3)]
            WT = [alloc([128, 256], BF16) for i in range(3)]
            xg_ = [alloc([128, 256], BF16) for i in range(3)]
            rtmp = alloc([128, D], F32)
            if l == 0:
                print("peer arena bytes", st['off'] * 2, "of", ARENA * 2)

            DMA('pool', wq, w_q[l].rearrange("(kc p) f -> p kc f", p=128), "wt", [], ["wq"])
            DMA('pool', ks, ksT[l], "wt2", [], ["ks"])

            passes = []
            t_ = 0
            if ntl % 2 == 1:
                passes.append([ntl - 1])
            while t_ + 1 < ntl:
                passes.append([t_, t_ + 1])
                t_ += 2
            PS = int(os.environ.get("K_PSMALL", "0"))
            if PS:
                passes = passes[:PS]

            def sel_front(pi):
                ptiles = passes[pi]
                nt = len(ptiles)
                N = nt * 128
                hb = pi % 2
                hk = "h2T%d" % hb
                for k, si in enumerate(ptiles):
                    DMA('sp', xt, xs[si * 128:(si + 1) * 128, :], "xl0", [], ["xt"])
                    j = 1 if si >= 17 else 0
                    norm_mod(xt, "xt", 1, j, h2T[hb][:, :, k * 128:(k + 1) * 128], hk, rtmp, xn, small, 7, sqkey="rtmp")
                    yield
                for ch in range(16):
                    o_ = banks[7][:, (ch % 2) * 256:(ch % 2) * 256 + N]
                    for kc in range(8):
                        MM(o_, wq[:, kc, ch * 128:(ch + 1) * 128], h2T[hb][:, kc, 0:N], kc == 0, kc == 7, ["wq", hk], [pk[7]])
                    CP('act', qpT[:, ch, 0:N], o_, [pk[7]], ["qpT"])
                    yield
                for k, si in enumerate(ptiles):
                    for q_ in range(4):
                        for g4 in range(4):
                            gi = q_ * 4 + g4
                            MM(banks[7][:, g4 * 128:(g4 + 1) * 128], qpT[:, gi, k * 128:(k + 1) * 128], ks[:, gi % 2, :],
                               True, True, ["qpT", "ks"], [pk[7]])
                        ACTV(big[:, q_ * 512:(q_ + 1) * 512], banks[7][:], AF.Identity, [pk[7]], ["big"])
                        yield
                    sv = big.rearrange("p (g k) -> p g k", k=128)
                    for gi in range(16):
                        S.op('dve', lambda e, gi=gi: e.max(out=vtop[:, gi, 0:8], in_=sv[:, gi, :]), ["big"], ["vtop"])
                        S.op('dve', lambda e, gi=gi: e.max_index(out=ix[:, gi, 0:8], in_max=vtop[:, gi, 0:8], in_values=sv[:, gi, :]),
                             ["big", "vtop"], ["ix"])
                        S.op('dve', lambda e, gi=gi: e.match_replace(out=wk[:, 0:128], in_to_replace=vtop[:, gi, 0:8],
                                                                      in_values=sv[:, gi, :], imm_value=-1e30),
                             ["big", "vtop"], ["wk"])
                        S.op('dve', lambda e, gi=gi: e.max(out=vtop[:, gi, 8:16], in_=wk[:, 0:128]), ["wk"], ["vtop"])
                        S.op('dve', lambda e, gi=gi: e.max_index(out=ix[:, gi, 8:16], in_max=vtop[:, gi, 8:16], in_values=wk[:, 0:128]),
                             ["wk", "vtop"], ["ix"])
                        yield
                    CP('dve', ixf, ix, ["ix"], ["ixf"])
                    v4 = vtop.rearrange("p (h t) k -> p h t k", t=2)
                    i4 = ixf.rearrange("p (h t) k -> p h t k", t=2)
                    cand = big.rearrange("p (h a b) -> p h a b", a=16, b=16)
                    TT('dve', cand, v4[:, :, 0, :].unsqueeze(3).to_broadcast([128, 8, 16, 16]),
                       v4[:, :, 1, :].unsqueeze(2).to_broadcast([128, 8, 16, 16]), ALU.add, ["vtop"], ["big"])
                    yield
                    c3 = big.rearrange("p (h c) -> p h c", c=256)
                    for h in range(8):
                        S.op('dve', lambda e, h=h: e.max(out=ts[:, h, 0:8], in_=c3[:, h, :]), ["big"], ["ts"])
                        S.op('dve', lambda e, h=h: e.max_index(out=pos[:, h, 0:8], in_max=ts[:, h, 0:8], in_values=c3[:, h, :]),
                             ["big", "ts"], ["pos"])
                        S.op('dve', lambda e, h=h: e.match_replace(out=wk, in_to_replace=ts[:, h, 0:8], in_values=c3[:, h, :],
                                                                    imm_value=-1e30), ["big", "ts"], ["wk"])
                        S.op('dve', lambda e, h=h: e.max(out=ts[:, h, 8:16], in_=wk), ["wk"], ["ts"])
                        S.op('dve', lambda e, h=h: e.max_index(out=pos[:, h, 8:16], in_max=ts[:, h, 8:16], in_values=wk),
                             ["wk", "ts"], ["pos"])
                        yield
                    TS('dve', pa, pos, 4, None, ALU.logical_shift_right, None, ["pos"], ["pa"])
                    TS('dve', pbq, pos, 15, None, ALU.bitwise_and, None, ["pos"], ["pbq"])
                    CP('dve', af, pa, ["pa"], ["af"])
                    CP('dve', bf, pbq, ["pbq"], ["bf"])
                    yield
                    oh = big.rearrange("p (h k a) -> p h k a", k=16, a=16)
                    io4 = iota16.rearrange("p (x y a) -> p x y a", x=1, y=1).to_broadcast([128, 8, 16, 16])
                    for (sel, idxv, dst) in [(af, i4[:, :, 0, :], e1f), (bf, i4[:, :, 1, :], e2f)]:
                        TT('dve', oh, sel.unsqueeze(3).to_broadcast([128, 8, 16, 16]), io4, ALU.is_equal,
                           ["af", "bf", "iota16"], ["big"])
                        TT('dve', oh, oh, idxv.unsqueeze(2).to_broadcast([128, 8, 16, 16]), ALU.mult, ["big", "ixf"], ["big"])
                        RSUM('dve', dst, oh, ["big"], ["e12"])
                        yield
                    TT('dve', gf, ts, ts[:, :, 0:1].to_broadcast([128, 8, 16]), ALU.subtract, ["ts"], ["gf"])
                    ACTV(gf, gf, AF.Exp, ["gf"], ["gf"])
                    RSUM('dve', zz[:, 0:8], gf, ["gf"], ["zz"])
                    S.op('dve', lambda e: e.reciprocal(zz[:, 8:16], zz[:, 0:8]), ["zz"], ["zz"])
                    TT('dve', gf, gf, zz[:, 8:16].unsqueeze(2).to_broadcast([128, 8, 16]), ALU.mult, ["gf", "zz"], ["gf"])
                    yield
                    for q_, srcv in enumerate([e1f, e2f, gf]):
                        TR(banks[7][:, q_ * 128:(q_ + 1) * 128], srcv.rearrange("p h k -> p (h k)"), identf,
                           ["e12", "gf", "identf"], [pk[7]])
                    CP('act', eT[:, :, k * 128:(k + 1) * 128], banks[7][:, 0:384].rearrange("p (q n) -> p q n", n=128), [pk[7]], ["eT"])
                    yield

            def sel_back(pi):
                ptiles = passes[pi]
                gidx = 0
                for k, si in enumerate(ptiles):
                    for grp in range(16):
                        n0 = k * 128 + grp * 8
                        w = gidx % 2
                        gidx += 1
                        Aoh, Boh = Aoh2[w], Boh2[w]
                        ka, kb = "Aoh%d" % w, "Boh%d" % w
                        ic = iota_cb.unsqueeze(1).to_broadcast([128, 8, 128])
                        TT('dve', Aoh, ic, eT[:, 0, n0:n0 + 8].unsqueeze(2).to_broadcast([128, 8, 128]), ALU.is_equal,
                           ["iota_cb", "eT"], [ka])
                        TT('dve', Boh, ic, eT[:, 1, n0:n0 + 8].unsqueeze(2).to_broadcast([128, 8, 128]), ALU.is_equal,
                           ["iota_cb", "eT"], [kb])
                        TT('dve', Boh, Boh, eT[:, 2, n0:n0 + 8].unsqueeze(2).to_broadcast([128, 8, 128]), ALU.mult,
                           [kb, "eT"], [kb])
                        for q2 in range(2):
                            b_ = 4 + 2 * w + q2
                            for tkn in range(4):
                                nn = q2 * 4 + tkn
                                MM(banks[b_][:, tkn * 128:(tkn + 1) * 128], Aoh[:, nn, :], Boh[:, nn, :], True, True,
                                   [ka, kb], [pk[b_]])
                            CP('act', GT[:, :, n0 + q2 * 4:n0 + q2 * 4 + 4],
                               banks[b_][:].rearrange("p (n j) -> p j n", j=128), [pk[b_]], ["GT"])

            def chunk_loop(pi, gen):
                ptiles = passes[pi]
                nt = len(ptiles)
                N = nt * 128
                hb = pi % 2
                hk = "h2T%d" % hb
                NG = 64
                NTB = len(ub)
                DEP = len(xsb)

                G0 = pi * NG
                GTOT = len(passes) * NG

                def load_tab(Gg):
                    b = Gg % NTB
                    gi_ = Gg % NG
                    DMA('pool', ub[b].rearrange("p j k c -> p j (k c)"),
                        uT[l, 2 * gi_:2 * gi_ + 2].rearrange("j p k c -> p j (k c)"), "tu%d" % b, [], ["ub%d" % b])
                    DMA('pool', vb[b], vT[l, 2 * gi_:2 * gi_ + 2].rearrange("j c d -> c j d"), "tv%d" % b, [], ["vb%d" % b])

                def u_mm(jc):
                    b = (G0 + jc // 2) % NTB
                    jj = jc % 2
                    sbk = 4 + jc % DEP
                    for kc in range(8):
                        MM(banks[sbk][:, 0:N], ub[b][:, jj, kc, :], h2T[hb][:, kc, 0:N], kc == 0, kc == 7,
                           ["ub%d" % b, hk], [pk[sbk]])

                if pi == 0:
                    for gi_ in range(min(NTB - 1, GTOT)):
                        load_tab(gi_)
                u_mm(0)
                u_mm(1)

                def front(jc):
                    w_ = jc % DEP
                    sbk = 4 + w_
                    xk, g1k, xgk = "xsb%d" % w_, "g1_%d" % w_, "xg%d" % w_
                    ACTV(xsb[w_][:, 0:N], banks[sbk][:, 0:N], AF.Identity, [pk[sbk]], [xk])
                    ACTV(g1_[w_][:, 0:N], banks[sbk][:, 0:N], AF.Square, [pk[sbk]], [g1k], scale=0.21145921592128275)
                    TT('pool', xg_[w_][:, 0:N], xsb[w_][:, 0:N], GT[:, jc, 0:N], ALU.mult, [xk, "GT"], [xgk])
                    STT('dve', g1_[w_][:, 0:N], g1_[w_][:, 0:N], 1.0, xsb[w_][:, 0:N], ALU.add, ALU.mult, [g1k, xk], [g1k])

                front(0)
                for jc in range(128):
                    gi_ = jc // 2
                    jj = jc % 2
                    b = (G0 + gi_) % NTB
                    w_ = jc % DEP
                    if jj == 0 and G0 + gi_ + NTB - 1 < GTOT:
                        load_tab(G0 + gi_ + NTB - 1)
                    if jc + 2 < 128:
                        u_mm(jc + 2)
                    if jc + 1 < 128:
                        front(jc + 1)
                    g1k, g2k, wtk, xgk = "g1_%d" % w_, "g2_%d" % w_, "WT%d" % w_, "xg%d" % w_
                    ACTV(g2_[w_][:, 0:N], g1_[w_][:, 0:N], AF.Sigmoid, [g1k], [g2k], scale=1.5957691216057308)
                    TT('dve', WT[w_][:, 0:N], xg_[w_][:, 0:N], g2_[w_][:, 0:N], ALU.mult, [xgk, g2k], [wtk])
                    for k in range(nt):
                        for dh in range(2):
                            ab = k * 2 + dh
                            MM(banks[ab][:], WT[w_][:, k * 128:(k + 1) * 128], vb[b][:, jj, dh * 512:(dh + 1) * 512],
                               jc == 0, jc == 127, [wtk, "vb%d" % b], [pk[ab]])
                    if gen is not None and jc >= 2:
                        next(gen, None)

            def residual(pi):
                ptiles = passes[pi]
                for k, si in enumerate(ptiles):
                    j = 1 if si >= 17 else 0
                    DMA('sp', xres, xs[si * 128:(si + 1) * 128, :], "xl1", [], ["xres"])
                    for dh in range(2):
                        ab = k * 2 + dh
                        TT('dve', rtmp[:, dh * 512:(dh + 1) * 512], banks[ab][:], gates[1][j][:, dh * 512:(dh + 1) * 512], ALU.mult,
                           [pk[ab], "gate1%d" % j], ["rtmp"])
                    TT('pool', rtmp, rtmp, xres, ALU.add, ["rtmp", "xres"], ["rtmp"])
                    dst = xs if l == 0 else out
                    DMA('sp', dst[si * 128:(si + 1) * 128, :], rtmp, "xst", ["rtmp"], [])

            def drain(gen):
                if gen is not None:
                    for _ in gen:
                        pass

            drain(sel_front(0))
            for pi in range(len(passes)):
                sel_back(pi)
                gen = sel_front(pi + 1) if pi + 1 < len(passes) else None
                chunk_loop(pi, gen)
                drain(gen)
                residual(pi)
            S.barrier()

        stop = int(os.environ.get("K_STOP", "99"))
        stage = 0
        for l in range(2):
            for ph in (mod_phase, attn_phase, peer_phase):
                if stage < stop:
                    ph(l)
                stage += 1
        if dbg is not None:
            if dbg[0] == "xs":
                DMA('sp', dbg_out, xs[dbg[1] * 128:(dbg[1] + 1) * 128, :], "misc", [], [])
            else:
                DMA('sp', dbg_out, arena[:, dbg[1] // 2:dbg[1] // 2 + 2048].bitcast(F32), "misc", [], [])
        S.barrier()
        S.emit()
    return nc


def rope_tables(pos):
    row = (pos // 64).astype(np.float32)
    col = (pos % 64).astype(np.float32)
    half = 64
    inv = (np.float32(10000.0) ** (-np.arange(0, half, 2, dtype=np.float32) / np.float32(half))).astype(np.float32)
    ang = np.concatenate([row[:, None] * inv, col[:, None] * inv], axis=-1).astype(np.float32)
    return np.cos(ang).astype(np.float32), np.sin(ang).astype(np.float32)


def make_in_maps(x, c, ctx, c_ctx, ada_w, ada_b, norm1_gain, norm2_gain, w_qkv, q_norm_gain,
                 k_norm_gain, w_o, attn_sinks, peer_w_q, peer_sub_keys, peer_u, peer_v):
    f = lambda a: np.ascontiguousarray(np.asarray(a, dtype=np.float32))
    x, c, ctx, c_ctx = f(x), f(c), f(ctx), f(c_ctx)
    shared = {}
    shared["ada_w"] = f(ada_w)
    ab = f(ada_b)
    shared["ada_b"] = ab
    shared["adabT"] = f(ab.reshape(2, 48, 128).transpose(0, 2, 1))
    shared["n1T"] = f(f(norm1_gain).reshape(2, 8, 128).transpose(0, 2, 1))
    shared["n2T"] = f(f(norm2_gain).reshape(2, 8, 128).transpose(0, 2, 1))
    shared["qkg"] = f(np.concatenate([np.tile(f(q_norm_gain), (1, 8)), np.tile(f(k_norm_gain), (1, 2))], axis=1))
    shared["w_qkv"] = f(w_qkv)
    shared["w_o"] = f(w_o)
    shared["peer_w_q"] = f(peer_w_q)
    shared["ksT"] = f(f(peer_sub_keys).transpose(0, 3, 1, 2))
    shared["uT"] = f(f(peer_u).reshape(2, 128, 128, 8, 128).transpose(0, 2, 4, 3, 1))
    shared["vT"] = f(f(peer_v).reshape(2, 128, 128, D).transpose(0, 2, 1, 3))
    shared["sinks"] = f(attn_sinks).reshape(1, 8)
    kk = np.arange(128)[:, None]
    qq = np.arange(128)[None, :]
    mprev = (qq <= kk).astype(np.float32)
    mnext = (kk <= qq).astype(np.float32)
    zero = np.zeros((128, 128), np.float32)
    in_maps = []
    for cid in range(8):
        b, half = cid // 2, cid % 2
        if half == 0:
            order = np.concatenate([np.arange(0, 2048), np.arange(2048, 2176), np.arange(2176, 4096)])
            mP0, mN15 = zero, mnext
        else:
            order = np.concatenate([np.arange(2048, 4096), np.arange(1920, 2048), np.arange(0, 1920)])
            mP0, mN15 = mprev, zero
        xin = np.concatenate([x[b][order], ctx[b]], axis=0)
        cos, sin = rope_tables(order)
        cosd = np.concatenate([cos, np.ones((256, 64), np.float32)], axis=0)
        sind = np.concatenate([sin, np.zeros((256, 64), np.float32)], axis=0)
        m = np.stack([np.tile(mm_, (1, 4)) for mm_ in (mprev, mnext, mP0, mN15)], axis=1)
        cT = np.stack([c[b].reshape(8, 128).T, c_ctx.reshape(8, 128).T], axis=2)
        d = dict(shared)
        d.update(xin=f(xin), cosd=f(cosd), sind=f(sind), maskd=f(m), cT=f(cT))
        in_maps.append(d)
    return in_maps


_NC = {}


def kernel(**inputs):
    in_maps = make_in_maps(**inputs)
    if "nc" not in _NC:
        _NC["nc"] = build()
    res = run_bass_kernel_spmd(_NC["nc"], in_maps, core_ids=list(range(8)))
    outp = np.zeros((4, 4096, D), np.float32)
    for cid in range(8):
        b, half = cid // 2, cid % 2
        outp[b, half * 2048:(half + 1) * 2048] = np.asarray(res.results[cid]["out"], dtype=np.float32)
    return outp
```

```python
import os
import numpy as np
import concourse.bass as bass
import concourse.mybir as mybir
from concourse.bass_utils import run_bass_kernel_spmd
from contextlib import ExitStack

F32 = mybir.dt.float32
BF16 = mybir.dt.bfloat16
U32 = mybir.dt.uint32
ALU = mybir.AluOpType
AF = mybir.ActivationFunctionType
AX = mybir.AxisListType

D = 1024
NT_IN = 34
NT_ST = 19
EPS = 1e-6
SCALE = 128.0 ** -0.5


class Sched:
    ENG = ('pe', 'act', 'dve', 'pool', 'sp')

    def __init__(self, nc, es):
        self.nc = nc
        self.es = es
        self.ops = {e: [] for e in self.ENG}
        self.lastw = {}
        self.readers = {}
        self.esem = {e: es.enter_context(nc.semaphore('s_' + e)) for e in self.ENG}
        self.dsem = {}

    def new_dsem(self, name):
        self.dsem[name] = [self.es.enter_context(self.nc.semaphore('d_' + name)), 0]
        return name

    def _deps(self, reads, writes):
        toks = []
        for k in reads:
            if k in self.lastw:
                toks.append(self.lastw[k])
        for k in writes:
            if k in self.lastw:
                toks.append(self.lastw[k])
            toks.extend(self.readers.get(k, {}).values())
        return toks

    def _commit(self, tok, reads, writes):
        for k in reads:
            self.readers.setdefault(k, {})[tok[1]] = tok
        for k in writes:
            self.lastw[k] = tok
            self.readers[k] = {}

    def op(self, eng, fn, reads=(), writes=()):
        toks = self._deps(reads, writes)
        tok = ('E', eng, len(self.ops[eng]))
        self.ops[eng].append(dict(fn=fn, deps=toks, ms=False, dsem=None))
        self._commit(tok, reads, writes)
        return tok

    def dma(self, eng, fns, sem, reads=(), writes=()):
        toks = self._deps(reads, writes)
        self.dsem[sem][1] += 16 * len(fns)
        tok = ('D', sem, self.dsem[sem][1])
        self.ops[eng].append(dict(fn=fns, deps=toks, ms=False, dsem=sem))
        self._commit(tok, reads, writes)
        return tok

    def wait_all(self, eng, toks):
        self.ops[eng].append(dict(fn=None, deps=list(toks), ms=False, dsem=None))

    def barrier(self):
        toks = []
        for e in self.ENG:
            for i in range(len(self.ops[e]) - 1, -1, -1):
                o = self.ops[e][i]
                if o['fn'] is not None and o['dsem'] is None:
                    toks.append(('E', e, i))
                    break
        for name, (h, cnt) in self.dsem.items():
            if cnt > 0:
                toks.append(('D', name, cnt))
        for e in self.ENG:
            self.wait_all(e, toks)
        self.lastw = {}
        self.readers = {}

    def emit(self):
        nc = self.nc
        for e in self.ENG:
            for o in self.ops[e]:
                nd = []
                for t in o['deps']:
                    if t[0] == 'E':
                        if t[1] == e and e == 'pe':
                            continue
                        self.ops[t[1]][t[2]]['ms'] = True
                    nd.append(t)
                o['deps'] = nd
        msval = {}
        for e in self.ENG:
            c = 0
            for i, o in enumerate(self.ops[e]):
                if o['ms']:
                    c += 1
                msval[(e, i)] = c

        def run(e, engobj):
            known = {}
            for i, o in enumerate(self.ops[e]):
                for t in o['deps']:
                    if t[0] == 'E':
                        s, v, h = t[1], msval[(t[1], t[2])], self.esem[t[1]]
                    else:
                        s, v, h = t[1], t[2], self.dsem[t[1]][0]
                    if known.get(s, 0) >= v:
                        continue
                    known[s] = v
                    engobj.wait_ge(h, v)
                if o['fn'] is None:
                    continue
                if o['dsem'] is not None:
                    for f in o['fn']:
                        f(engobj).then_inc(self.dsem[o['dsem']][0], 16)
                else:
                    ins = o['fn'](engobj)
                    if o['ms']:
                        ins.then_inc(self.esem[e], 1)

        with nc.Block() as block:
            @block.tensor
            def _(t):
                run('pe', t)

            @block.scalar
            def _(t):
                run('act', t)

            @block.vector
            def _(t):
                run('dve', t)

            @block.gpsimd
            def _(t):
                run('pool', t)

            @block.sync
            def _(t):
                run('sp', t)


def build(dbg=None):
    nc = bass.Bass("TRN2", target_bir_lowering=False)

    def din(name, shape):
        return nc.dram_tensor(name, shape, F32, kind="ExternalInput").ap()

    xin = din("xin", [NT_IN * 128, D])
    cosd = din("cosd", [NT_IN * 128, 64])
    sind = din("sind", [NT_IN * 128, 64])
    maskd = din("maskd", [128, 4, 512])
    cTd = din("cT", [128, 8, 2])
    ada_w = din("ada_w", [2, D, 6 * D])
    adabT = din("adabT", [2, 128, 48])
    ada_b = din("ada_b", [2, 6 * D])
    n1T = din("n1T", [2, 128, 8])
    n2T = din("n2T", [2, 128, 8])
    qkg = din("qkg", [2, 1280])
    w_qkv = din("w_qkv", [2, D, 1536])
    w_o = din("w_o", [2, D, D])
    w_q = din("peer_w_q", [2, D, 2048])
    ksT = din("ksT", [2, 128, 2, 128])
    uT = din("uT", [2, 128, 128, 8, 128])
    vT = din("vT", [2, 128, 128, D])
    sinks = din("sinks", [1, 8])
    out = nc.dram_tensor("out", [2048, D], F32, kind="ExternalOutput").ap()
    xs = nc.dram_tensor("xs", [NT_ST * 128, D], F32, kind="Internal").ap()
    dbg_out = None
    if dbg is not None:
        dbg_out = nc.dram_tensor("dbg", [128, 1024], F32, kind="ExternalOutput").ap()

    with ExitStack() as es:
        S = Sched(nc, es)
        ARENA = 206 * 1024 // 2
        arena = es.enter_context(nc.sbuf_tensor("arena", [128, ARENA], BF16))
        banks = [es.enter_context(nc.psum_tensor("pb%d" % i, [128, 512], F32)) for i in range(8)]
        pk = ["ps%d" % i for i in range(8)]
        st = dict(off=0)

        def alloc(shape, dt):
            shape = list(shape[1:])
            n = int(np.prod(shape))
            units = n * (2 if dt in (F32, U32) else 1)
            a = st['off']
            st['off'] = a + units + (units % 2)
            assert st['off'] <= ARENA, ("arena overflow", st['off'] * 2)
            v = arena[:, a:a + units]
            if dt != BF16:
                v = v.bitcast(dt)
            if len(shape) > 1:
                names = "abcd"[:len(shape)]
                kw = {names[i]: shape[i] for i in range(len(shape))}
                v = v.rearrange("p (%s) -> p %s" % (" ".join(names), " ".join(names)), **kw)
            return v

        def MM(o, lhsT, rhs, start, stop, reads, writes):
            S.op('pe', lambda e: e.matmul(o, lhsT=lhsT, rhs=rhs, start=start, stop=stop), reads, writes)

        def TR(o, in_, idn, reads, writes):
            S.op('pe', lambda e: e.transpose(o, in_, idn), reads, writes)

        def ACTV(o, in_, func, reads, writes, scale=None, bias=None, eng='act'):
            kw = {}
            if scale is not None:
                kw['scale'] = scale
            if bias is not None:
                kw['bias'] = bias
            S.op(eng, lambda e: e.activation(out=o, in_=in_, func=func, **kw), reads, writes)

        def TT(eng, o, a, b, op, reads, writes):
            S.op(eng, lambda e: e.tensor_tensor(out=o, in0=a, in1=b, op=op), reads, writes)

        def TS(eng, o, a, s1, s2, op0, op1, reads, writes):
            if s2 is None:
                S.op(eng, lambda e: e.tensor_single_scalar(out=o, in_=a, scalar=s1, op=op0), reads, writes)
            else:
                S.op(eng, lambda e: e.tensor_scalar(out=o, in0=a, scalar1=s1, scalar2=s2, op0=op0, op1=op1), reads, writes)

        def STT(eng, o, a, sc, b, op0, op1, reads, writes):
            S.op(eng, lambda e: e.scalar_tensor_tensor(out=o, in0=a, scalar=sc, in1=b, op0=op0, op1=op1), reads, writes)

        def CP(eng, o, a, reads, writes):
            if eng == 'act':
                S.op(eng, lambda e: e.activation(out=o, in_=a, func=AF.Identity), reads, writes)
            else:
                S.op(eng, lambda e: e.tensor_copy(out=o, in_=a), reads, writes)

        def RSUM(eng, o, a, reads, writes):
            S.op(eng, lambda e: e.tensor_reduce(out=o, in_=a, axis=AX.X, op=ALU.add), reads, writes)

        def MEMSET(eng, o, val, writes):
            S.op(eng, lambda e: e.memset(o, val), (), writes)

        mctr = [0]

        def DMA(eng, o, i, sem, reads, writes):
            if sem == "misc":
                sem = "misc%d" % (mctr[0] % 12)
                mctr[0] += 1
            return S.dma(eng, [lambda e: e.dma_start(out=o, in_=i)], sem, reads, writes)

        def psbf(b):
            return banks[b][:].bitcast(BF16)

        ident = alloc([128, 128], BF16)
        identf = alloc([128, 128], F32)
        iota_c = alloc([128, 128], F32)
        iota16 = alloc([128, 16], F32)
        iota_cb = alloc([128, 128], BF16)
        esink = alloc([128, 8], F32)
        scT = alloc([128, 8, 2], F32)
        modT = alloc([128, 6, 8, 2], F32)
        AB = alloc([128, 2, 2, 2, 8], F32)
        gates = [[alloc([128, D], F32) for j in range(2)] for m in range(2)]
        nT = alloc([128, 2, 8], F32)
        abT = alloc([128, 48], F32)
        epsb = alloc([128, 1], F32)
        PBASE = st['off']

        for n_ in ["ld_a", "ld_b", "ld_c", "ld_d", "wt", "xl0", "xl1", "rp0", "rp1", "xst", "tu0", "tu1", "tv0", "tv1", "tu2", "tv2",
                   "xr0", "xr1", "wt2"] + ["misc%d" % i for i in range(12)]:
            S.new_dsem(n_)

        S.op('pool', lambda e: e.iota(identf, [[1, 128]], base=0, channel_multiplier=-1,
                                      allow_small_or_imprecise_dtypes=True), (), ["identf"])
        TS('dve', identf, identf, 0.0, None, ALU.is_equal, None, ["identf"], ["identf"])
        CP('dve', ident, identf, ["identf"], ["ident"])
        S.op('pool', lambda e: e.iota(iota_c, [[1, 128]], base=0, channel_multiplier=0,
                                      allow_small_or_imprecise_dtypes=True), (), ["iota_c"])
        S.op('pool', lambda e: e.iota(iota16, [[1, 16]], base=0, channel_multiplier=0,
                                      allow_small_or_imprecise_dtypes=True), (), ["iota16"])
        MEMSET('dve', epsb, EPS, ["epsb"])
        CP('dve', iota_cb, iota_c, ["iota_c"], ["iota_cb"])
        DMA('sp', scT, cTd, "misc", [], ["scT"])
        DMA('sp', esink, sinks.partition_broadcast(128), "misc", [], ["esink"])
        ACTV(esink, esink, AF.Exp, ["esink"], ["esink"])
        ACTV(scT, scT, AF.Silu, ["scT"], ["scT"])

        def mod_phase(l):
            st['off'] = PBASE
            crep = [alloc([128, 8, 128], F32) for j in range(2)]
            piece = [alloc([128, 8, D], F32) for i in range(2)]
            bcb = alloc([128, D], F32)
            for j in range(2):
                CP('dve', crep[j], scT[:, :, j:j + 1].to_broadcast([128, 8, 128]), ["scT"], ["crep%d" % j])
            DMA('sp', abT, adabT[l], "misc", [], ["abT"])
            DMA('sp', nT[:, 0, :], n1T[l], "misc", [], ["nT"])
            DMA('sp', nT[:, 1, :], n2T[l], "misc", [], ["nT"])
            semn = ["ld_a", "ld_b"]
            for m in range(6):
                pc = piece[m % 2]
                pkey = "piece%d" % (m % 2)
                DMA('sp', pc, ada_w[l, :, m * D:(m + 1) * D].rearrange("(kc p) f -> p kc f", p=128), semn[m % 2], [], [pkey])
                if m in (0, 1, 3, 4):
                    pb = banks[m % 2]
                    pv = pb[:, 0:16].rearrange("p (f j) -> p f j", j=2)
                    for fc in range(8):
                        for kc in range(8):
                            MM(pv[:, fc, :], pc[:, kc, fc * 128:(fc + 1) * 128], scT[:, kc, :], kc == 0, kc == 7,
                               [pkey, "scT"], [pk[m % 2]])
                    TT('dve', modT[:, m], pv, abT[:, m * 8:(m + 1) * 8].unsqueeze(2).to_broadcast([128, 8, 2]), ALU.add,
                       [pk[m % 2], "abT"], ["modT%d" % m])
                else:
                    gi = 0 if m == 2 else 1
                    DMA('sp', bcb, ada_b[l:l + 1, m * D:(m + 1) * D].partition_broadcast(128), "ld_c", [], ["bcb"])
                    for j in range(2):
                        for dh in range(2):
                            b_ = 2 + (j * 2 + dh) % 2
                            for kc in range(8):
                                MM(banks[b_][:], crep[j][:, kc, :], pc[:, kc, dh * 512:(dh + 1) * 512], kc == 0, kc == 7,
                                   [pkey, "crep%d" % j], [pk[b_]])
                            TT('dve', gates[gi][j][:, dh * 512:(dh + 1) * 512], banks[b_][:], bcb[:, dh * 512:(dh + 1) * 512],
                               ALU.add, [pk[b_], "bcb"], ["gate%d%d" % (gi, j)])
            for ni, (msh, msc) in enumerate([(0, 1), (3, 4)]):
                for j in range(2):
                    TS('dve', AB[:, ni, 0, j, :], modT[:, msc, :, j], 1.0, None, ALU.add, None, ["modT%d" % msc], ["AB"])
                    TT('dve', AB[:, ni, 0, j, :], AB[:, ni, 0, j, :], nT[:, ni, :], ALU.mult, ["AB", "nT"], ["AB"])
                    CP('dve', AB[:, ni, 1, j, :], modT[:, msh, :, j], ["modT%d" % msh], ["AB"])
            S.barrier()

        def norm_mod(*a, **k):
            for _ in norm_mod_g(*a, **k):
                pass

        def norm_mod_g(xt, xkey, ni, j, hT_view, hkey, sq, xn, small, bank, sqkey="sq", smallkey="small", evac_act=False):
            ACTV(sq, xt, AF.Square, [xkey], [sqkey])
            RSUM('dve', small[:, 0:1], sq, [sqkey], [smallkey])
            yield
            ACTV(small[:, 1:2], small[:, 0:1], AF.Sqrt, [smallkey, "epsb"], [smallkey], scale=1.0 / D, bias=epsb)
            S.op('dve', lambda e: e.reciprocal(small[:, 2:3], small[:, 1:2]), [smallkey], [smallkey])
            yield
            ACTV(xn, xt, AF.Identity, [xkey, smallkey], ["xn"], scale=small[:, 2:3])
            pv = psbf(bank).rearrange("p (k n) -> p k n", n=128)
            for kc in range(8):
                TR(pv[:, kc, :], xn[:, kc * 128:(kc + 1) * 128], ident, ["xn", "ident"], [pk[bank]])
            yield
            for kc in range(8):
                if evac_act:
                    ACTV(hT_view[:, kc, :], pv[:, kc, :], AF.Identity, [pk[bank], "AB"], [hkey],
                         scale=AB[:, ni, 0, j, kc:kc + 1], bias=AB[:, ni, 1, j, kc:kc + 1])
                else:
                    TS('dve', hT_view[:, kc, :], pv[:, kc, :], AB[:, ni, 0, j, kc:kc + 1], AB[:, ni, 1, j, kc:kc + 1],
                       ALU.mult, ALU.add, [pk[bank], "AB"], [hkey])

        def attn_phase(l):
            st['off'] = PBASE
            kT = alloc([128, 2, NT_IN, 128], BF16)
            Vst = alloc([128, NT_IN, 2, 130], BF16)
            qT = alloc([128, 8, NT_ST, 128], BF16)
            PT = [alloc([128, 512], BF16) for i in range(2)]
            wqkv = alloc([128, 8, 1536], BF16)
            wo = alloc([128, 8, D], BF16)
            xt = [alloc([128, D], F32) for i in range(2)]
            xn = alloc([128, D], BF16)
            hT = [alloc([128, 8, 128], BF16) for i in range(2)]
            ybuf = [alloc([128, 10, 128], F32) for i in range(2)]
            tmpAB = [alloc([128, 1280], F32) for i in range(2)]
            tmpA = [tt_[:, 0:640].rearrange("p (h a f) -> p h a f", a=2, f=32) for tt_ in tmpAB]
            tmpB = [tt_[:, 640:1280].rearrange("p (h a f) -> p h a f", a=2, f=32) for tt_ in tmpAB]
            qkbuf = [alloc([128, 10, 128], BF16) for i in range(2)]
            smallb = [alloc([128, 32], F32) for i in range(2)]
            cs = [[alloc([128, 2, 32], F32) for k in range(2)] for i in range(2)]
            small = alloc([128, 32], F32)
            ob = alloc([128, 8, 128], BF16)
            oT = alloc([128, 8, 128], BF16)
            rtmp = alloc([128, D], F32)
            den = alloc([128, 8], F32)
            masks = alloc([128, 4, 512], F32)
            qkgb = alloc([128, 10, 128], F32)
            DMA('sp', masks, maskd, "misc", [], ["masks"])
            DMA('sp', qkgb.rearrange("p a b -> p (a b)"), qkg[l:l + 1, :].partition_broadcast(128), "misc", [], ["qkgb"])

            DMA('pool', wqkv, w_qkv[l].rearrange("(kc p) f -> p kc f", p=128), "wt", [], ["wqkv"])
            DMA('pool', wo, w_o[l].rearrange("(kc p) f -> p kc f", p=128), "wt2", [], ["wo"])
            MEMSET('pool', Vst[:, :, :, 128:130], 1.0, ["Vst"])

            if l == 0:
                tiles = [(t, xin, t, (t if t <= 16 else (t - 15 if t >= 32 else None))) for t in range(NT_IN)]
            else:
                tiles = [(si, xs, si, (si if si <= 15 else None)) for si in range(17)] + \
                        [(32, xs, 17, None), (33, xs, 18, None)]
            SMALL = int(os.environ.get("K_SMALL", "0"))
            CUT = int(os.environ.get("K_CUT", "99"))
            if SMALL:
                keep = {0, 1, 15, 16, 17, 32, 33}
                tiles = [tt for tt in tiles if tt[0] in keep]

            def load_tile(i):
                t, src, stile, qi = tiles[i]
                b = i % 2
                DMA('sp', xt[b], src[stile * 128:(stile + 1) * 128, :], "xl%d" % b, [], ["xt%d" % b])

            def load_rope(i):
                t, src, stile, qi = tiles[i]
                b = i % 2
                ri = t
                S.dma('sp', [lambda e: e.dma_start(out=cs[b][0].rearrange("p a b -> p (a b)"), in_=cosd[ri * 128:(ri + 1) * 128, :]),
                             lambda e: e.dma_start(out=cs[b][1].rearrange("p a b -> p (a b)"), in_=sind[ri * 128:(ri + 1) * 128, :])],
                      "rp%d" % b, [], ["cs%d" % b])

            load_tile(0)
            load_rope(0)

            def stage1(i):
                t, src, stile, qi = tiles[i]
                b = i % 2
                if i + 1 < len(tiles):
                    load_tile(i + 1)
                j = 1 if t >= 32 else 0
                y, ysq, qk, small = ybuf[b], tmpAB[b].rearrange("p (h d) -> p h d", d=128), qkbuf[b], smallb[b]
                tmp = [tmpA[b], tmpB[b]]
                KY, KQ, KS, KT0, KT1 = "y%d" % b, "qk%d" % b, "small%d" % b, "tmpA%d" % b, "tmpB%d" % b
                yield from norm_mod_g(xt[b], "xt%d" % b, 0, j, hT[b], "hT%d" % b, rtmp, xn, small, 3, sqkey="rtmp", smallkey="smalln%d" % b, evac_act=True)

            def stage1b(i):
                t, src, stile, qi = tiles[i]
                b = i % 2
                y = ybuf[b]
                KY = "y%d" % b
                cbs = [0, 1, 2] if qi is not None else [2]
                for cb in cbs:
                    for kc in range(8):
                        MM(banks[cb][:], hT[b][:, kc, :], wqkv[:, kc, cb * 512:(cb + 1) * 512], kc == 0, kc == 7,
                           ["hT%d" % b, "wqkv"], [pk[cb]])
                nh = 10 if qi is not None else 2
                h0 = 0 if qi is not None else 8
                yv = y[:, h0:10, :]
                yield
                SUB = os.environ.get("K_SUB", "abc")
                if "b" not in SUB:
                    return
                if qi is not None:
                    ACTV(y[:, 0:4, :], banks[0][:].rearrange("p (h d) -> p h d", d=128), AF.Identity, [pk[0]], [KY])
                    ACTV(y[:, 4:8, :], banks[1][:].rearrange("p (h d) -> p h d", d=128), AF.Identity, [pk[1]], [KY])
                ACTV(y[:, 8:10, :], banks[2][:, 0:256].rearrange("p (h d) -> p h d", d=128), AF.Identity, [pk[2]], [KY])
                if "c" not in SUB:
                    return
                CP('act', Vst[:, t, :, 0:128], banks[2][:, 256:512].rearrange("p (g d) -> p g d", d=128), [pk[2]], ["Vst"])

            def stage2(i):
                t, src, stile, qi = tiles[i]
                b = i % 2
                if i + 1 < len(tiles):
                    load_rope(i + 1)
                j = 1 if t >= 32 else 0
                y, ysq, qk, small = ybuf[b], tmpAB[b].rearrange("p (h d) -> p h d", d=128), qkbuf[b], smallb[b]
                tmp = [tmpA[b], tmpB[b]]
                KY, KQ, KS, KT0, KT1 = "y%d" % b, "qk%d" % b, "small%d" % b, "tmpA%d" % b, "tmpB%d" % b
                nh = 10 if qi is not None else 2
                h0 = 0 if qi is not None else 8
                yv = y[:, h0:10, :]
                if CUT < 3:
                    return
                ACTV(ysq[:, h0:10, :], yv, AF.Square, [KY], [KT0, KT1])
                RSUM('dve', small[:, 8 + h0:18], ysq[:, h0:10, :], [KT0, KT1], [KS])
                yield
                ACTV(small[:, 8 + h0:18], small[:, 8 + h0:18], AF.Sqrt, [KS, "epsb"], [KS], scale=1.0 / 128, bias=epsb)
                yield
                S.op('dve', lambda e, h0=h0, small=small: e.reciprocal(small[:, 8 + h0:18], small[:, 8 + h0:18]), [KS], [KS])
                TT('dve', yv, yv, small[:, 8 + h0:18].unsqueeze(2).to_broadcast([128, nh, 128]), ALU.mult, [KY, KS], [KY])
                TT('dve', yv, yv, qkgb[:, h0:10, :], ALU.mult, [KY, "qkgb"], [KY])
                if CUT < 4:
                    return
                y4 = y.rearrange("p h (a t f) -> p h a t f", a=2, t=2)
                q4 = qk.rearrange("p h (a t f) -> p h a t f", a=2, t=2)
                x1 = y4[:, h0:10, :, 0, :]
                x2 = y4[:, h0:10, :, 1, :]
                cb_ = cs[b][0].unsqueeze(1).to_broadcast([128, nh, 2, 32])
                sb_ = cs[b][1].unsqueeze(1).to_broadcast([128, nh, 2, 32])
                t0 = tmp[0][:, h0:10]
                t1 = tmp[1][:, h0:10]
                ck = "cs%d" % b
                TT('dve', t0, x1, cb_, ALU.mult, [KY, ck], [KT0])
                TT('dve', t1, x2, sb_, ALU.mult, [KY, ck], [KT1])
                TT('dve', q4[:, h0:10, :, 0, :], t0, t1, ALU.subtract, [KT0, KT1], [KQ])
                TT('dve', t0, x1, sb_, ALU.mult, [KY, ck], [KT0])
                TT('dve', t1, x2, cb_, ALU.mult, [KY, ck], [KT1])
                TT('dve', q4[:, h0:10, :, 1, :], t0, t1, ALU.add, [KT0, KT1], [KQ])
                yield
                if CUT < 5:
                    return
                if qi is not None:
                    pv = psbf(4).rearrange("p (h n) -> p h n", n=128)
                    for h in range(8):
                        TR(pv[:, h, :], qk[:, h, :], ident, [KQ, "ident"], [pk[4]])
                    CP('act', qT[:, :, qi, :], pv, [pk[4]], ["qT"])
                pv2 = psbf(5).rearrange("p (h n) -> p h n", n=128)
                for g in range(2):
                    TR(pv2[:, g, :], qk[:, 8 + g, :], ident, [KQ, "ident"], [pk[5]])
                yield
                CP('act', kT[:, :, t, :], pv2[:, 0:2, :], [pk[5]], ["kT"])


            def rr(gens):
                gens = [g_ for g_ in gens if g_ is not None]
                while gens:
                    for g_ in list(gens):
                        try:
                            next(g_)
                        except StopIteration:
                            gens.remove(g_)

            rr([stage1(0)])
            if len(tiles) > 1:
                rr([stage1(1), stage1b(0)])
            else:
                rr([stage1b(0)])
            for i in range(len(tiles)):
                rr([stage2(i),
                    stage1b(i + 1) if i + 1 < len(tiles) else None,
                    stage1(i + 2) if i + 2 < len(tiles) else None])

            if l == 0:
                qtiles = list(range(NT_ST))
            else:
                qtiles = list(range(16))

            def load_res(i):
                si = qtiles[i]
                b = i % 2
                if l == 0:
                    stile = si if si <= 16 else si + 15
                    src = xin
                else:
                    stile, src = si, xs
                DMA('sp', xt[b], src[stile * 128:(stile + 1) * 128, :], "xl%d" % b, [], ["xt%d" % b])

            if SMALL:
                qtiles = [qq_ for qq_ in qtiles if qq_ in (0, 15, 17)][:SMALL]
            if CUT < 6:
                qtiles = []
            if qtiles:
                load_res(0)
            pti = 0
            for i, si in enumerate(qtiles):
                b = i % 2
                if i + 1 < len(qtiles):
                    load_res(i + 1)
                isctx = si >= 17
                if l == 0:
                    keys = [(32, None), (33, None)] if isctx else [(t, None) for t in range(NT_IN)]
                else:
                    prev = (si - 1, 0) if si > 0 else (16, 2)
                    nxt = (si + 1, 1) if si < 15 else (16, 3)
                    keys = [prev, (si, None), nxt, (32, None), (33, None)]
                if SMALL:
                    keys = [kk_ for kk_ in keys if kk_[0] in keep]
                for g in range(2):
                    nk = len(keys)
                    base = pti
                    pti += nk

                    def emit_st(ki, g=g, base=base, si=si, keys=keys):
                        t = keys[ki][0]
                        sb_i = 4 + ((base + ki) % 2)
                        MM(banks[sb_i][:].rearrange("p (r q) -> p r q", q=128), kT[:, g, t, :], qT[:, 4 * g:4 * g + 4, si, :], True, True,
                           ["kT", "qT"], [pk[sb_i]])

                    emit_st(0)
                    for ki, (t, mi) in enumerate(keys):
                        sb_i = 4 + ((base + ki) % 2)
                        pt = PT[(base + ki) % 2]
                        ptk = "PT%d" % ((base + ki) % 2)
                        if ki + 1 < nk:
                            emit_st(ki + 1)
                        ACTV(pt, banks[sb_i][:], AF.Exp, [pk[sb_i]], [ptk], scale=SCALE)
                        if mi is not None:
                            TT('dve', pt, pt, masks[:, mi, :], ALU.mult, [ptk, "masks"], [ptk])
                        if CUT < 7:
                            continue
                        for r in range(4):
                            MM(banks[r][:, 0:129], pt[:, r * 128:(r + 1) * 128], Vst[:, t, g, 0:129], ki == 0, ki == nk - 1,
                               [ptk, "Vst"], [pk[r]])
                    if CUT < 8:
                        continue
                    for r in range(4):
                        h = 4 * g + r
                        if l == 1:
                            TT('dve', den[:, h:h + 1], banks[r][:, 128:129], esink[:, h:h + 1], ALU.add, [pk[r], "esink"], ["den"])
                        else:
                            CP('dve', den[:, h:h + 1], banks[r][:, 128:129], [pk[r]], ["den"])
                        S.op('dve', lambda e, h=h: e.reciprocal(den[:, h:h + 1], den[:, h:h + 1]), ["den"], ["den"])
                        ACTV(ob[:, h, :], banks[r][:, 0:128], AF.Identity, [pk[r], "den"], ["ob"], scale=den[:, h:h + 1])
                if CUT < 9:
                    continue
                pv = psbf(6).rearrange("p (h n) -> p h n", n=128)
                for h in range(8):
                    TR(pv[:, h, :], ob[:, h, :], ident, ["ob", "ident"], [pk[6]])
                CP('dve', oT, pv, [pk[6]], ["oT"])
                j = 1 if isctx else 0
                for dh in range(2):
                    for h in range(8):
                        MM(banks[7][:], oT[:, h, :], wo[:, h, dh * 512:(dh + 1) * 512], h == 0, h == 7, ["oT", "wo"], [pk[7]])
                    TT('dve', rtmp[:, dh * 512:(dh + 1) * 512], banks[7][:], gates[0][j][:, dh * 512:(dh + 1) * 512], ALU.mult,
                       [pk[7], "gate0%d" % j], ["rtmp"])
                TT('pool', rtmp, rtmp, xt[b], ALU.add, ["rtmp", "xt%d" % b], ["rtmp"])
                DMA('sp', xs[si * 128:(si + 1) * 128, :], rtmp, "xst", ["rtmp"], [])
            S.barrier()

        def peer_phase(l):
            st['off'] = PBASE
            ntl = NT_ST if l == 0 else 16
            wq = alloc([128, 8, 2048], BF16)
            ks = alloc([128, 2, 128], BF16)
            GT = alloc([128, 128, 256], BF16)
            ub = [alloc([128, 2, 8, 128], BF16) for i in range(3)]
            vb = [alloc([128, 2, D], BF16) for i in range(3)]
            big = alloc([128, 2048], F32)
            wk = alloc([128, 256], F32)
            vtop = alloc([128, 16, 16], F32)
            ix = alloc([128, 16, 16], U32)
            ixf = alloc([128, 16, 16], F32)
            ts = alloc([128, 8, 16], F32)
            pos = alloc([128, 8, 16], U32)
            pa = alloc([128, 8, 16], U32)
            pbq = alloc([128, 8, 16], U32)
            af = alloc([128, 8, 16], F32)
            bf = alloc([128, 8, 16], F32)
            e1f = alloc([128, 8, 16], F32)
            e2f = alloc([128, 8, 16], F32)
            gf = alloc([128, 8, 16], F32)
            zz = alloc([128, 16], F32)
            eT = alloc([128, 3, 256], BF16)
            AB2 = [alloc([128, 2, 8, 128], BF16) for i in range(2)]
            Aoh2 = [t_[:, 0] for t_ in AB2]
            Boh2 = [t_[:, 1] for t_ in AB2]
            h2T = [alloc([128, 8, 256], BF16) for i in range(2)]
            qpT = alloc([128, 16, 256], BF16)
            xt = alloc([128, D], F32)
            xres = alloc([128, D], F32)
            xn = alloc([128, D], BF16)
            small = alloc([128, 32], F32)
            xsb = [alloc([128, 256], F32) for i in range(3)]
            g1_ = [alloc([128, 256], F32) for i in range(3)]
            g2_ = [alloc([128, 256], BF16) for i in range(3)]
            WT = [alloc([128, 256], BF16) for i in range(3)]
            xg_ = [alloc([128, 256], BF16) for i in range(3)]
            rtmp = alloc([128, D], F32)
            if l == 0:
                print("peer arena bytes", st['off'] * 2, "of", ARENA * 2)

            DMA('pool', wq, w_q[l].rearrange("(kc p) f -> p kc f", p=128), "wt", [], ["wq"])
            DMA('pool', ks, ksT[l], "wt2", [], ["ks"])

            passes = []
            t_ = 0
            if ntl % 2 == 1:
                passes.append([ntl - 1])
            while t_ + 1 < ntl:
                passes.append([t_, t_ + 1])
                t_ += 2
            PS = int(os.environ.get("K_PSMALL", "0"))
            if PS:
                passes = passes[:PS]

            def sel_front(pi):
                ptiles = passes[pi]
                nt = len(ptiles)
                N = nt * 128
                hb = pi % 2
                hk = "h2T%d" % hb
                for k, si in enumerate(ptiles):
                    DMA('sp', xt, xs[si * 128:(si + 1) * 128, :], "xl0", [], ["xt"])
                    j = 1 if si >= 17 else 0
                    norm_mod(xt, "xt", 1, j, h2T[hb][:, :, k * 128:(k + 1) * 128], hk, rtmp, xn, small, 7, sqkey="rtmp")
                    yield
                for ch in range(16):
                    o_ = banks[7][:, (ch % 2) * 256:(ch % 2) * 256 + N]
                    for kc in range(8):
                        MM(o_, wq[:, kc, ch * 128:(ch + 1) * 128], h2T[hb][:, kc, 0:N], kc == 0, kc == 7, ["wq", hk], [pk[7]])
                    CP('act', qpT[:, ch, 0:N], o_, [pk[7]], ["qpT"])
                    yield
                for k, si in enumerate(ptiles):
                    for q_ in range(4):
                        for g4 in range(4):
                            gi = q_ * 4 + g4
                            MM(banks[7][:, g4 * 128:(g4 + 1) * 128], qpT[:, gi, k * 128:(k + 1) * 128], ks[:, gi % 2, :],
                               True, True, ["qpT", "ks"], [pk[7]])
                        ACTV(big[:, q_ * 512:(q_ + 1) * 512], banks[7][:], AF.Identity, [pk[7]], ["big"])
                        yield
                    sv = big.rearrange("p (g k) -> p g k", k=128)
                    for gi in range(16):
                        S.op('dve', lambda e, gi=gi: e.max(out=vtop[:, gi, 0:8], in_=sv[:, gi, :]), ["big"], ["vtop"])
                        S.op('dve', lambda e, gi=gi: e.max_index(out=ix[:, gi, 0:8], in_max=vtop[:, gi, 0:8], in_values=sv[:, gi, :]),
                             ["big", "vtop"], ["ix"])
                        S.op('dve', lambda e, gi=gi: e.match_replace(out=wk[:, 0:128], in_to_replace=vtop[:, gi, 0:8],
                                                                      in_values=sv[:, gi, :], imm_value=-1e30),
                             ["big", "vtop"], ["wk"])
                        S.op('dve', lambda e, gi=gi: e.max(out=vtop[:, gi, 8:16], in_=wk[:, 0:128]), ["wk"], ["vtop"])
                        S.op('dve', lambda e, gi=gi: e.max_index(out=ix[:, gi, 8:16], in_max=vtop[:, gi, 8:16], in_values=wk[:, 0:128]),
                             ["wk", "vtop"], ["ix"])
                        yield
                    CP('dve', ixf, ix, ["ix"], ["ixf"])
                    v4 = vtop.rearrange("p (h t) k -> p h t k", t=2)
                    i4 = ixf.rearrange("p (h t) k -> p h t k", t=2)
                    cand = big.rearrange("p (h a b) -> p h a b", a=16, b=16)
                    TT('dve', cand, v4[:, :, 0, :].unsqueeze(3).to_broadcast([128, 8, 16, 16]),
                       v4[:, :, 1, :].unsqueeze(2).to_broadcast([128, 8, 16, 16]), ALU.add, ["vtop"], ["big"])
                    yield
                    c3 = big.rearrange("p (h c) -> p h c", c=256)
                    for h in range(8):
                        S.op('dve', lambda e, h=h: e.max(out=ts[:, h, 0:8], in_=c3[:, h, :]), ["big"], ["ts"])
                        S.op('dve', lambda e, h=h: e.max_index(out=pos[:, h, 0:8], in_max=ts[:, h, 0:8], in_values=c3[:, h, :]),
                             ["big", "ts"], ["pos"])
                        S.op('dve', lambda e, h=h: e.match_replace(out=wk, in_to_replace=ts[:, h, 0:8], in_values=c3[:, h, :],
                                                                    imm_value=-1e30), ["big", "ts"], ["wk"])
                        S.op('dve', lambda e, h=h: e.max(out=ts[:, h, 8:16], in_=wk), ["wk"], ["ts"])
                        S.op('dve', lambda e, h=h: e.max_index(out=pos[:, h, 8:16], in_max=ts[:, h, 8:16], in_values=wk),
                             ["wk", "ts"], ["pos"])
                        yield
                    TS('dve', pa, pos, 4, None, ALU.logical_shift_right, None, ["pos"], ["pa"])
                    TS('dve', pbq, pos, 15, None, ALU.bitwise_and, None, ["pos"], ["pbq"])
                    CP('dve', af, pa, ["pa"], ["af"])
                    CP('dve', bf, pbq, ["pbq"], ["bf"])
                    yield
                    oh = big.rearrange("p (h k a) -> p h k a", k=16, a=16)
                    io4 = iota16.rearrange("p (x y a) -> p x y a", x=1, y=1).to_broadcast([128, 8, 16, 16])
                    for (sel, idxv, dst) in [(af, i4[:, :, 0, :], e1f), (bf, i4[:, :, 1, :], e2f)]:
                        TT('dve', oh, sel.unsqueeze(3).to_broadcast([128, 8, 16, 16]), io4, ALU.is_equal,
                           ["af", "bf", "iota16"], ["big"])
                        TT('dve', oh, oh, idxv.unsqueeze(2).to_broadcast([128, 8, 16, 16]), ALU.mult, ["big", "ixf"], ["big"])
                        RSUM('dve', dst, oh, ["big"], ["e12"])
                        yield
                    TT('dve', gf, ts, ts[:, :, 0:1].to_broadcast([128, 8, 16]), ALU.subtract, ["ts"], ["gf"])
                    ACTV(gf, gf, AF.Exp, ["gf"], ["gf"])
                    RSUM('dve', zz[:, 0:8], gf, ["gf"], ["zz"])
                    S.op('dve', lambda e: e.reciprocal(zz[:, 8:16], zz[:, 0:8]), ["zz"], ["zz"])
                    TT('dve', gf, gf, zz[:, 8:16].unsqueeze(2).to_broadcast([128, 8, 16]), ALU.mult, ["gf", "zz"], ["gf"])
                    yield
                    for q_, srcv in enumerate([e1f, e2f, gf]):
                        TR(banks[7][:, q_ * 128:(q_ + 1) * 128], srcv.rearrange("p h k -> p (h k)"), identf,
                           ["e12", "gf", "identf"], [pk[7]])
                    CP('act', eT[:, :, k * 128:(k + 1) * 128], banks[7][:, 0:384].rearrange("p (q n) -> p q n", n=128), [pk[7]], ["eT"])
                    yield

            def sel_back(pi):
                ptiles = passes[pi]
                gidx = 0
                for k, si in enumerate(ptiles):
                    for grp in range(16):
                        n0 = k * 128 + grp * 8
                        w = gidx % 2
                        gidx += 1
                        Aoh, Boh = Aoh2[w], Boh2[w]
                        ka, kb = "Aoh%d" % w, "Boh%d" % w
                        ic2 = iota_cb.rearrange("p (x y c) -> p x y c", x=1, y=1).to_broadcast([128, 2, 8, 128])
                        TT('dve', AB2[w], ic2, eT[:, 0:2, n0:n0 + 8].unsqueeze(3).to_broadcast([128, 2, 8, 128]), ALU.is_equal,
                           ["iota_cb", "eT"], [ka, kb])
                        TT('dve', Boh, Boh, eT[:, 2, n0:n0 + 8].unsqueeze(2).to_broadcast([128, 8, 128]), ALU.mult,
                           [kb, "eT"], [kb])
                        for q2 in range(2):
                            b_ = 4 + 2 * w + q2
                            for tkn in range(4):
                                nn = q2 * 4 + tkn
                                MM(banks[b_][:, tkn * 128:(tkn + 1) * 128], Aoh[:, nn, :], Boh[:, nn, :], True, True,
                                   [ka, kb], [pk[b_]])
                            CP('act', GT[:, :, n0 + q2 * 4:n0 + q2 * 4 + 4],
                               banks[b_][:].rearrange("p (n j) -> p j n", j=128), [pk[b_]], ["GT"])

            def chunk_loop(pi, gen):
                ptiles = passes[pi]
                nt = len(ptiles)
                N = nt * 128
                hb = pi % 2
                hk = "h2T%d" % hb
                NG = 64
                NTB = len(ub)
                DEP = len(xsb)

                def load_tab(gi_):
                    b = gi_ % NTB
                    DMA('pool', ub[b].rearrange("p j k c -> p j (k c)"),
                        uT[l, 2 * gi_:2 * gi_ + 2].rearrange("j p k c -> p j (k c)"), "tu%d" % b, [], ["ub%d" % b])
                    DMA('pool', vb[b], vT[l, 2 * gi_:2 * gi_ + 2].rearrange("j c d -> c j d"), "tv%d" % b, [], ["vb%d" % b])

                def u_mm(jc):
                    b = (jc // 2) % NTB
                    jj = jc % 2
                    sbk = 4 + jc % DEP
                    for kc in range(8):
                        MM(banks[sbk][:, 0:N], ub[b][:, jj, kc, :], h2T[hb][:, kc, 0:N], kc == 0, kc == 7,
                           ["ub%d" % b, hk], [pk[sbk]])

                for gi_ in range(min(NTB - 1, NG)):
                    load_tab(gi_)
                u_mm(0)
                u_mm(1)

                def front(jc):
                    w_ = jc % DEP
                    sbk = 4 + w_
                    xk, g1k, xgk = "xsb%d" % w_, "g1_%d" % w_, "xg%d" % w_
                    ACTV(xsb[w_][:, 0:N], banks[sbk][:, 0:N], AF.Identity, [pk[sbk]], [xk])
                    ACTV(g1_[w_][:, 0:N], banks[sbk][:, 0:N], AF.Square, [pk[sbk]], [g1k], scale=0.21145921592128275)
                    TT('pool', xg_[w_][:, 0:N], xsb[w_][:, 0:N], GT[:, jc, 0:N], ALU.mult, [xk, "GT"], [xgk])
                    STT('dve', g1_[w_][:, 0:N], g1_[w_][:, 0:N], 1.0, xsb[w_][:, 0:N], ALU.add, ALU.mult, [g1k, xk], [g1k])

                front(0)
                for jc in range(128):
                    gi_ = jc // 2
                    jj = jc % 2
                    b = gi_ % NTB
                    w_ = jc % DEP
                    if jj == 0 and gi_ + NTB - 1 < NG:
                        load_tab(gi_ + NTB - 1)
                    if jc + 2 < 128:
                        u_mm(jc + 2)
                    if jc + 1 < 128:
                        front(jc + 1)
                    g1k, g2k, wtk, xgk = "g1_%d" % w_, "g2_%d" % w_, "WT%d" % w_, "xg%d" % w_
                    ACTV(g2_[w_][:, 0:N], g1_[w_][:, 0:N], AF.Sigmoid, [g1k], [g2k], scale=1.5957691216057308)
                    TT('dve', WT[w_][:, 0:N], xg_[w_][:, 0:N], g2_[w_][:, 0:N], ALU.mult, [xgk, g2k], [wtk])
                    for k in range(nt):
                        for dh in range(2):
                            ab = k * 2 + dh
                            MM(banks[ab][:], WT[w_][:, k * 128:(k + 1) * 128], vb[b][:, jj, dh * 512:(dh + 1) * 512],
                               jc == 0, jc == 127, [wtk, "vb%d" % b], [pk[ab]])
                    if gen is not None and jc >= 2:
                        next(gen, None)

            def residual(pi):
                ptiles = passes[pi]
                for k, si in enumerate(ptiles):
                    j = 1 if si >= 17 else 0
                    DMA('sp', xres, xs[si * 128:(si + 1) * 128, :], "xl1", [], ["xres"])
                    for dh in range(2):
                        ab = k * 2 + dh
                        TT('dve', rtmp[:, dh * 512:(dh + 1) * 512], banks[ab][:], gates[1][j][:, dh * 512:(dh + 1) * 512], ALU.mult,
                           [pk[ab], "gate1%d" % j], ["rtmp"])
                    TT('pool', rtmp, rtmp, xres, ALU.add, ["rtmp", "xres"], ["rtmp"])
                    dst = xs if l == 0 else out
                    DMA('sp', dst[si * 128:(si + 1) * 128, :], rtmp, "xst", ["rtmp"], [])

            def drain(gen):
                if gen is not None:
                    for _ in gen:
                        pass

            drain(sel_front(0))
            for pi in range(len(passes)):
                sel_back(pi)
                gen = sel_front(pi + 1) if pi + 1 < len(passes) else None
                chunk_loop(pi, gen)
                drain(gen)
                residual(pi)
            S.barrier()

        stop = int(os.environ.get("K_STOP", "99"))
        stage = 0
        for l in range(2):
            for ph in (mod_phase, attn_phase, peer_phase):
                if stage < stop:
                    ph(l)
                stage += 1
        if dbg is not None:
            if dbg[0] == "xs":
                DMA('sp', dbg_out, xs[dbg[1] * 128:(dbg[1] + 1) * 128, :], "misc", [], [])
            else:
                DMA('sp', dbg_out, arena[:, dbg[1] // 2:dbg[1] // 2 + 2048].bitcast(F32), "misc", [], [])
        S.barrier()
        S.emit()
    return nc


def rope_tables(pos):
    row = (pos // 64).astype(np.float32)
    col = (pos % 64).astype(np.float32)
    half = 64
    inv = (np.float32(10000.0) ** (-np.arange(0, half, 2, dtype=np.float32) / np.float32(half))).astype(np.float32)
    ang = np.concatenate([row[:, None] * inv, col[:, None] * inv], axis=-1).astype(np.float32)
    return np.cos(ang).astype(np.float32), np.sin(ang).astype(np.float32)


def make_in_maps(x, c, ctx, c_ctx, ada_w, ada_b, norm1_gain, norm2_gain, w_qkv, q_norm_gain,
                 k_norm_gain, w_o, attn_sinks, peer_w_q, peer_sub_keys, peer_u, peer_v):
    f = lambda a: np.ascontiguousarray(np.asarray(a, dtype=np.float32))
    x, c, ctx, c_ctx = f(x), f(c), f(ctx), f(c_ctx)
    shared = {}
    shared["ada_w"] = f(ada_w)
    ab = f(ada_b)
    shared["ada_b"] = ab
    shared["adabT"] = f(ab.reshape(2, 48, 128).transpose(0, 2, 1))
    shared["n1T"] = f(f(norm1_gain).reshape(2, 8, 128).transpose(0, 2, 1))
    shared["n2T"] = f(f(norm2_gain).reshape(2, 8, 128).transpose(0, 2, 1))
    shared["qkg"] = f(np.concatenate([np.tile(f(q_norm_gain), (1, 8)), np.tile(f(k_norm_gain), (1, 2))], axis=1))
    shared["w_qkv"] = f(w_qkv)
    shared["w_o"] = f(w_o)
    shared["peer_w_q"] = f(peer_w_q)
    shared["ksT"] = f(f(peer_sub_keys).transpose(0, 3, 1, 2))
    shared["uT"] = f(f(peer_u).reshape(2, 128, 128, 8, 128).transpose(0, 2, 4, 3, 1))
    shared["vT"] = f(f(peer_v).reshape(2, 128, 128, D).transpose(0, 2, 1, 3))
    shared["sinks"] = f(attn_sinks).reshape(1, 8)
    kk = np.arange(128)[:, None]
    qq = np.arange(128)[None, :]
    mprev = (qq <= kk).astype(np.float32)
    mnext = (kk <= qq).astype(np.float32)
    zero = np.zeros((128, 128), np.float32)
    in_maps = []
    for cid in range(8):
        b, half = cid // 2, cid % 2
        if half == 0:
            order = np.concatenate([np.arange(0, 2048), np.arange(2048, 2176), np.arange(2176, 4096)])
            mP0, mN15 = zero, mnext
        else:
            order = np.concatenate([np.arange(2048, 4096), np.arange(1920, 2048), np.arange(0, 1920)])
            mP0, mN15 = mprev, zero
        xin = np.concatenate([x[b][order], ctx[b]], axis=0)
        cos, sin = rope_tables(order)
        cosd = np.concatenate([cos, np.ones((256, 64), np.float32)], axis=0)
        sind = np.concatenate([sin, np.zeros((256, 64), np.float32)], axis=0)
        m = np.stack([np.tile(mm_, (1, 4)) for mm_ in (mprev, mnext, mP0, mN15)], axis=1)
        cT = np.stack([c[b].reshape(8, 128).T, c_ctx.reshape(8, 128).T], axis=2)
        d = dict(shared)
        d.update(xin=f(xin), cosd=f(cosd), sind=f(sind), maskd=f(m), cT=f(cT))
        in_maps.append(d)
    return in_maps


_NC = {}


def kernel(**inputs):
    in_maps = make_in_maps(**inputs)
    if "nc" not in _NC:
        _NC["nc"] = build()
    res = run_bass_kernel_spmd(_NC["nc"], in_maps, core_ids=list(range(8)))
    outp = np.zeros((4, 4096, D), np.float32)
    for cid in range(8):
        b, half = cid // 2, cid % 2
        outp[b, half * 2048:(half + 1) * 2048] = np.asarray(res.results[cid]["out"], dtype=np.float32)
    return outp
```

```python
import os
import numpy as np
import concourse.bass as bass
import concourse.mybir as mybir
from concourse.bass_utils import run_bass_kernel_spmd
from contextlib import ExitStack

F32 = mybir.dt.float32
BF16 = mybir.dt.bfloat16
U32 = mybir.dt.uint32
ALU = mybir.AluOpType
AF = mybir.ActivationFunctionType
AX = mybir.AxisListType

D = 1024
NT_IN = 34
NT_ST = 19
EPS = 1e-6
SCALE = 128.0 ** -0.5


class Sched:
    ENG = ('pe', 'act', 'dve', 'pool', 'sp')

    def __init__(self, nc, es):
        self.nc = nc
        self.es = es
        self.ops = {e: [] for e in self.ENG}
        self.lastw = {}
        self.readers = {}
        self.esem = {e: es.enter_context(nc.semaphore('s_' + e)) for e in self.ENG}
        self.dsem = {}

    def new_dsem(self, name):
        self.dsem[name] = [self.es.enter_context(self.nc.semaphore('d_' + name)), 0]
        return name

    def _deps(self, reads, writes):
        toks = []
        for k in reads:
            if k in self.lastw:
                toks.append(self.lastw[k])
        for k in writes:
            if k in self.lastw:
                toks.append(self.lastw[k])
            toks.extend(self.readers.get(k, {}).values())
        return toks

    def _commit(self, tok, reads, writes):
        for k in reads:
            self.readers.setdefault(k, {})[tok[1]] = tok
        for k in writes:
            self.lastw[k] = tok
            self.readers[k] = {}

    def op(self, eng, fn, reads=(), writes=()):
        toks = self._deps(reads, writes)
        tok = ('E', eng, len(self.ops[eng]))
        self.ops[eng].append(dict(fn=fn, deps=toks, ms=False, dsem=None))
        self._commit(tok, reads, writes)
        return tok

    def dma(self, eng, fns, sem, reads=(), writes=()):
        toks = self._deps(reads, writes)
        self.dsem[sem][1] += 16 * len(fns)
        tok = ('D', sem, self.dsem[sem][1])
        self.ops[eng].append(dict(fn=fns, deps=toks, ms=False, dsem=sem))
        self._commit(tok, reads, writes)
        return tok

    def wait_all(self, eng, toks):
        self.ops[eng].append(dict(fn=None, deps=list(toks), ms=False, dsem=None))

    def barrier(self):
        toks = []
        for e in self.ENG:
            for i in range(len(self.ops[e]) - 1, -1, -1):
                o = self.ops[e][i]
                if o['fn'] is not None and o['dsem'] is None:
                    toks.append(('E', e, i))
                    break
        for name, (h, cnt) in self.dsem.items():
            if cnt > 0:
                toks.append(('D', name, cnt))
        for e in self.ENG:
            self.wait_all(e, toks)
        self.lastw = {}
        self.readers = {}

    def emit(self):
        nc = self.nc
        for e in self.ENG:
            for o in self.ops[e]:
                nd = []
                for t in o['deps']:
                    if t[0] == 'E':
                        if t[1] == e and e == 'pe':
                            continue
                        self.ops[t[1]][t[2]]['ms'] = True
                    nd.append(t)
                o['deps'] = nd
        msval = {}
        for e in self.ENG:
            c = 0
            for i, o in enumerate(self.ops[e]):
                if o['ms']:
                    c += 1
                msval[(e, i)] = c

        def run(e, engobj):
            known = {}
            for i, o in enumerate(self.ops[e]):
                for t in o['deps']:
                    if t[0] == 'E':
                        s, v, h = t[1], msval[(t[1], t[2])], self.esem[t[1]]
                    else:
                        s, v, h = t[1], t[2], self.dsem[t[1]][0]
                    if known.get(s, 0) >= v:
                        continue
                    known[s] = v
                    engobj.wait_ge(h, v)
                if o['fn'] is None:
                    continue
                if o['dsem'] is not None:
                    for f in o['fn']:
                        f(engobj).then_inc(self.dsem[o['dsem']][0], 16)
                else:
                    ins = o['fn'](engobj)
                    if o['ms']:
                        ins.then_inc(self.esem[e], 1)

        with nc.Block() as block:
            @block.tensor
            def _(t):
                run('pe', t)

            @block.scalar
            def _(t):
                run('act', t)

            @block.vector
            def _(t):
                run('dve', t)

            @block.gpsimd
            def _(t):
                run('pool', t)

            @block.sync
            def _(t):
                run('sp', t)


def build(dbg=None):
    nc = bass.Bass("TRN2", target_bir_lowering=False)

    def din(name, shape):
        return nc.dram_tensor(name, shape, F32, kind="ExternalInput").ap()

    xin = din("xin", [NT_IN * 128, D])
    cosd = din("cosd", [NT_IN * 128, 64])
    sind = din("sind", [NT_IN * 128, 64])
    maskd = din("maskd", [128, 4, 512])
    cTd = din("cT", [128, 8, 2])
    ada_w = din("ada_w", [2, D, 6 * D])
    adabT = din("adabT", [2, 128, 48])
    ada_b = din("ada_b", [2, 6 * D])
    n1T = din("n1T", [2, 128, 8])
    n2T = din("n2T", [2, 128, 8])
    qkg = din("qkg", [2, 1280])
    w_qkv = din("w_qkv", [2, D, 1536])
    w_o = din("w_o", [2, D, D])
    w_q = din("peer_w_q", [2, D, 2048])
    ksT = din("ksT", [2, 128, 2, 128])
    uT = din("uT", [2, 128, 128, 8, 128])
    vT = din("vT", [2, 128, 128, D])
    sinks = din("sinks", [1, 8])
    out = nc.dram_tensor("out", [2048, D], F32, kind="ExternalOutput").ap()
    xs = nc.dram_tensor("xs", [NT_ST * 128, D], F32, kind="Internal").ap()
    dbg_out = None
    if dbg is not None:
        dbg_out = nc.dram_tensor("dbg", [128, 1024], F32, kind="ExternalOutput").ap()

    with ExitStack() as es:
        S = Sched(nc, es)
        ARENA = 206 * 1024 // 2
        arena = es.enter_context(nc.sbuf_tensor("arena", [128, ARENA], BF16))
        banks = [es.enter_context(nc.psum_tensor("pb%d" % i, [128, 512], F32)) for i in range(8)]
        pk = ["ps%d" % i for i in range(8)]
        st = dict(off=0)

        def alloc(shape, dt):
            shape = list(shape[1:])
            n = int(np.prod(shape))
            units = n * (2 if dt in (F32, U32) else 1)
            a = st['off']
            st['off'] = a + units + (units % 2)
            assert st['off'] <= ARENA, ("arena overflow", st['off'] * 2)
            v = arena[:, a:a + units]
            if dt != BF16:
                v = v.bitcast(dt)
            if len(shape) > 1:
                names = "abcd"[:len(shape)]
                kw = {names[i]: shape[i] for i in range(len(shape))}
                v = v.rearrange("p (%s) -> p %s" % (" ".join(names), " ".join(names)), **kw)
            return v

        def MM(o, lhsT, rhs, start, stop, reads, writes):
            S.op('pe', lambda e: e.matmul(o, lhsT=lhsT, rhs=rhs, start=start, stop=stop), reads, writes)

        def TR(o, in_, idn, reads, writes):
            S.op('pe', lambda e: e.transpose(o, in_, idn), reads, writes)

        def ACTV(o, in_, func, reads, writes, scale=None, bias=None, eng='act'):
            kw = {}
            if scale is not None:
                kw['scale'] = scale
            if bias is not None:
                kw['bias'] = bias
            S.op(eng, lambda e: e.activation(out=o, in_=in_, func=func, **kw), reads, writes)

        def TT(eng, o, a, b, op, reads, writes):
            S.op(eng, lambda e: e.tensor_tensor(out=o, in0=a, in1=b, op=op), reads, writes)

        def TS(eng, o, a, s1, s2, op0, op1, reads, writes):
            if s2 is None:
                S.op(eng, lambda e: e.tensor_single_scalar(out=o, in_=a, scalar=s1, op=op0), reads, writes)
            else:
                S.op(eng, lambda e: e.tensor_scalar(out=o, in0=a, scalar1=s1, scalar2=s2, op0=op0, op1=op1), reads, writes)

        def STT(eng, o, a, sc, b, op0, op1, reads, writes):
            S.op(eng, lambda e: e.scalar_tensor_tensor(out=o, in0=a, scalar=sc, in1=b, op0=op0, op1=op1), reads, writes)

        def CP(eng, o, a, reads, writes):
            if eng == 'act':
                S.op(eng, lambda e: e.activation(out=o, in_=a, func=AF.Identity), reads, writes)
            else:
                S.op(eng, lambda e: e.tensor_copy(out=o, in_=a), reads, writes)

        def RSUM(eng, o, a, reads, writes):
            S.op(eng, lambda e: e.tensor_reduce(out=o, in_=a, axis=AX.X, op=ALU.add), reads, writes)

        def MEMSET(eng, o, val, writes):
            S.op(eng, lambda e: e.memset(o, val), (), writes)

        mctr = [0]

        def DMA(eng, o, i, sem, reads, writes):
            if sem == "misc":
                sem = "misc%d" % (mctr[0] % 12)
                mctr[0] += 1
            return S.dma(eng, [lambda e: e.dma_start(out=o, in_=i)], sem, reads, writes)

        def psbf(b):
            return banks[b][:].bitcast(BF16)

        ident = alloc([128, 128], BF16)
        identf = alloc([128, 128], F32)
        iota_c = alloc([128, 128], F32)
        iota16 = alloc([128, 16], F32)
        iota_cb = alloc([128, 128], BF16)
        esink = alloc([128, 8], F32)
        scT = alloc([128, 8, 2], F32)
        modT = alloc([128, 6, 8, 2], F32)
        AB = alloc([128, 2, 2, 2, 8], F32)
        gates = [[alloc([128, D], F32) for j in range(2)] for m in range(2)]
        nT = alloc([128, 2, 8], F32)
        abT = alloc([128, 48], F32)
        epsb = alloc([128, 1], F32)
        PBASE = st['off']

        for n_ in ["ld_a", "ld_b", "ld_c", "ld_d", "wt", "xl0", "xl1", "rp0", "rp1", "xst", "tu0", "tu1", "tv0", "tv1", "tu2", "tv2",
                   "xr0", "xr1", "wt2"] + ["misc%d" % i for i in range(12)]:
            S.new_dsem(n_)

        S.op('pool', lambda e: e.iota(identf, [[1, 128]], base=0, channel_multiplier=-1,
                                      allow_small_or_imprecise_dtypes=True), (), ["identf"])
        TS('dve', identf, identf, 0.0, None, ALU.is_equal, None, ["identf"], ["identf"])
        CP('dve', ident, identf, ["identf"], ["ident"])
        S.op('pool', lambda e: e.iota(iota_c, [[1, 128]], base=0, channel_multiplier=0,
                                      allow_small_or_imprecise_dtypes=True), (), ["iota_c"])
        S.op('pool', lambda e: e.iota(iota16, [[1, 16]], base=0, channel_multiplier=0,
                                      allow_small_or_imprecise_dtypes=True), (), ["iota16"])
        MEMSET('dve', epsb, EPS, ["epsb"])
        CP('dve', iota_cb, iota_c, ["iota_c"], ["iota_cb"])
        DMA('sp', scT, cTd, "misc", [], ["scT"])
        DMA('sp', esink, sinks.partition_broadcast(128), "misc", [], ["esink"])
        ACTV(esink, esink, AF.Exp, ["esink"], ["esink"])
        ACTV(scT, scT, AF.Silu, ["scT"], ["scT"])

        def mod_phase(l):
            st['off'] = PBASE
            crep = [alloc([128, 8, 128], F32) for j in range(2)]
            piece = [alloc([128, 8, D], F32) for i in range(2)]
            bcb = alloc([128, D], F32)
            for j in range(2):
                CP('dve', crep[j], scT[:, :, j:j + 1].to_broadcast([128, 8, 128]), ["scT"], ["crep%d" % j])
            DMA('sp', abT, adabT[l], "misc", [], ["abT"])
            DMA('sp', nT[:, 0, :], n1T[l], "misc", [], ["nT"])
            DMA('sp', nT[:, 1, :], n2T[l], "misc", [], ["nT"])
            semn = ["ld_a", "ld_b"]
            for m in range(6):
                pc = piece[m % 2]
                pkey = "piece%d" % (m % 2)
                DMA('sp', pc, ada_w[l, :, m * D:(m + 1) * D].rearrange("(kc p) f -> p kc f", p=128), semn[m % 2], [], [pkey])
                if m in (0, 1, 3, 4):
                    pb = banks[m % 2]
                    pv = pb[:, 0:16].rearrange("p (f j) -> p f j", j=2)
                    for fc in range(8):
                        for kc in range(8):
                            MM(pv[:, fc, :], pc[:, kc, fc * 128:(fc + 1) * 128], scT[:, kc, :], kc == 0, kc == 7,
                               [pkey, "scT"], [pk[m % 2]])
                    TT('dve', modT[:, m], pv, abT[:, m * 8:(m + 1) * 8].unsqueeze(2).to_broadcast([128, 8, 2]), ALU.add,
                       [pk[m % 2], "abT"], ["modT%d" % m])
                else:
                    gi = 0 if m == 2 else 1
                    DMA('sp', bcb, ada_b[l:l + 1, m * D:(m + 1) * D].partition_broadcast(128), "ld_c", [], ["bcb"])
                    for j in range(2):
                        for dh in range(2):
                            b_ = 2 + (j * 2 + dh) % 2
                            for kc in range(8):
                                MM(banks[b_][:], crep[j][:, kc, :], pc[:, kc, dh * 512:(dh + 1) * 512], kc == 0, kc == 7,
                                   [pkey, "crep%d" % j], [pk[b_]])
                            TT('dve', gates[gi][j][:, dh * 512:(dh + 1) * 512], banks[b_][:], bcb[:, dh * 512:(dh + 1) * 512],
                               ALU.add, [pk[b_], "bcb"], ["gate%d%d" % (gi, j)])
            for ni, (msh, msc) in enumerate([(0, 1), (3, 4)]):
                for j in range(2):
                    TS('dve', AB[:, ni, 0, j, :], modT[:, msc, :, j], 1.0, None, ALU.add, None, ["modT%d" % msc], ["AB"])
                    TT('dve', AB[:, ni, 0, j, :], AB[:, ni, 0, j, :], nT[:, ni, :], ALU.mult, ["AB", "nT"], ["AB"])
                    CP('dve', AB[:, ni, 1, j, :], modT[:, msh, :, j], ["modT%d" % msh], ["AB"])
            S.barrier()

        def norm_mod(*a, **k):
            for _ in norm_mod_g(*a, **k):
                pass

        def norm_mod_g(xt, xkey, ni, j, hT_view, hkey, sq, xn, small, bank, sqkey="sq", smallkey="small", evac_act=False):
            ACTV(sq, xt, AF.Square, [xkey], [sqkey])
            RSUM('dve', small[:, 0:1], sq, [sqkey], [smallkey])
            yield
            ACTV(small[:, 1:2], small[:, 0:1], AF.Sqrt, [smallkey, "epsb"], [smallkey], scale=1.0 / D, bias=epsb)
            S.op('dve', lambda e: e.reciprocal(small[:, 2:3], small[:, 1:2]), [smallkey], [smallkey])
            yield
            ACTV(xn, xt, AF.Identity, [xkey, smallkey], ["xn"], scale=small[:, 2:3])
            pv = psbf(bank).rearrange("p (k n) -> p k n", n=128)
            for kc in range(8):
                TR(pv[:, kc, :], xn[:, kc * 128:(kc + 1) * 128], ident, ["xn", "ident"], [pk[bank]])
            yield
            for kc in range(8):
                if evac_act:
                    ACTV(hT_view[:, kc, :], pv[:, kc, :], AF.Identity, [pk[bank], "AB"], [hkey],
                         scale=AB[:, ni, 0, j, kc:kc + 1], bias=AB[:, ni, 1, j, kc:kc + 1])
                else:
                    TS('dve', hT_view[:, kc, :], pv[:, kc, :], AB[:, ni, 0, j, kc:kc + 1], AB[:, ni, 1, j, kc:kc + 1],
                       ALU.mult, ALU.add, [pk[bank], "AB"], [hkey])

        def attn_phase(l):
            st['off'] = PBASE
            kT = alloc([128, 2, NT_IN, 128], BF16)
            Vst = alloc([128, NT_IN, 2, 130], BF16)
            qT = alloc([128, 8, NT_ST, 128], BF16)
            PT = [alloc([128, 512], BF16) for i in range(2)]
            wqkv = alloc([128, 8, 1536], BF16)
            wo = alloc([128, 8, D], BF16)
            xt = [alloc([128, D], F32) for i in range(2)]
            xn = alloc([128, D], BF16)
            hT = [alloc([128, 8, 128], BF16) for i in range(2)]
            ybuf = [alloc([128, 10, 128], F32) for i in range(2)]
            tmpAB = [alloc([128, 1280], F32) for i in range(2)]
            tmpA = [tt_[:, 0:640].rearrange("p (h a f) -> p h a f", a=2, f=32) for tt_ in tmpAB]
            tmpB = [tt_[:, 640:1280].rearrange("p (h a f) -> p h a f", a=2, f=32) for tt_ in tmpAB]
            qkbuf = [alloc([128, 10, 128], BF16) for i in range(2)]
            smallb = [alloc([128, 32], F32) for i in range(2)]
            cs = [[alloc([128, 2, 32], F32) for k in range(2)] for i in range(2)]
            small = alloc([128, 32], F32)
            ob = alloc([128, 8, 128], BF16)
            oT = alloc([128, 8, 128], BF16)
            rtmp = alloc([128, D], F32)
            den = alloc([128, 8], F32)
            masks = alloc([128, 4, 512], F32)
            qkgb = alloc([128, 10, 128], F32)
            DMA('sp', masks, maskd, "misc", [], ["masks"])
            DMA('sp', qkgb.rearrange("p a b -> p (a b)"), qkg[l:l + 1, :].partition_broadcast(128), "misc", [], ["qkgb"])

            DMA('pool', wqkv, w_qkv[l].rearrange("(kc p) f -> p kc f", p=128), "wt", [], ["wqkv"])
            DMA('pool', wo, w_o[l].rearrange("(kc p) f -> p kc f", p=128), "wt2", [], ["wo"])
            MEMSET('pool', Vst[:, :, :, 128:130], 1.0, ["Vst"])

            if l == 0:
                tiles = [(t, xin, t, (t if t <= 16 else (t - 15 if t >= 32 else None))) for t in range(NT_IN)]
            else:
                tiles = [(si, xs, si, (si if si <= 15 else None)) for si in range(17)] + \
                        [(32, xs, 17, None), (33, xs, 18, None)]
            SMALL = int(os.environ.get("K_SMALL", "0"))
            CUT = int(os.environ.get("K_CUT", "99"))
            if SMALL:
                keep = {0, 1, 15, 16, 17, 32, 33}
                tiles = [tt for tt in tiles if tt[0] in keep]

            def load_tile(i):
                t, src, stile, qi = tiles[i]
                b = i % 2
                DMA('sp', xt[b], src[stile * 128:(stile + 1) * 128, :], "xl%d" % b, [], ["xt%d" % b])

            def load_rope(i):
                t, src, stile, qi = tiles[i]
                b = i % 2
                ri = t
                S.dma('sp', [lambda e: e.dma_start(out=cs[b][0].rearrange("p a b -> p (a b)"), in_=cosd[ri * 128:(ri + 1) * 128, :]),
                             lambda e: e.dma_start(out=cs[b][1].rearrange("p a b -> p (a b)"), in_=sind[ri * 128:(ri + 1) * 128, :])],
                      "rp%d" % b, [], ["cs%d" % b])

            load_tile(0)
            load_rope(0)

            def stage1(i):
                t, src, stile, qi = tiles[i]
                b = i % 2
                if i + 1 < len(tiles):
                    load_tile(i + 1)
                j = 1 if t >= 32 else 0
                y, ysq, qk, small = ybuf[b], tmpAB[b].rearrange("p (h d) -> p h d", d=128), qkbuf[b], smallb[b]
                tmp = [tmpA[b], tmpB[b]]
                KY, KQ, KS, KT0, KT1 = "y%d" % b, "qk%d" % b, "small%d" % b, "tmpA%d" % b, "tmpB%d" % b
                yield from norm_mod_g(xt[b], "xt%d" % b, 0, j, hT[b], "hT%d" % b, rtmp, xn, small, 3, sqkey="rtmp", smallkey="smalln%d" % b, evac_act=True)

            def stage1b(i):
                t, src, stile, qi = tiles[i]
                b = i % 2
                y = ybuf[b]
                KY = "y%d" % b
                cbs = [0, 1, 2] if qi is not None else [2]
                for cb in cbs:
                    for kc in range(8):
                        MM(banks[cb][:], hT[b][:, kc, :], wqkv[:, kc, cb * 512:(cb + 1) * 512], kc == 0, kc == 7,
                           ["hT%d" % b, "wqkv"], [pk[cb]])
                nh = 10 if qi is not None else 2
                h0 = 0 if qi is not None else 8
                yv = y[:, h0:10, :]
                yield
                SUB = os.environ.get("K_SUB", "abc")
                if "b" not in SUB:
                    return
                if qi is not None:
                    ACTV(y[:, 0:4, :], banks[0][:].rearrange("p (h d) -> p h d", d=128), AF.Identity, [pk[0]], [KY])
                    ACTV(y[:, 4:8, :], banks[1][:].rearrange("p (h d) -> p h d", d=128), AF.Identity, [pk[1]], [KY])
                ACTV(y[:, 8:10, :], banks[2][:, 0:256].rearrange("p (h d) -> p h d", d=128), AF.Identity, [pk[2]], [KY])
                if "c" not in SUB:
                    return
                CP('act', Vst[:, t, :, 0:128], banks[2][:, 256:512].rearrange("p (g d) -> p g d", d=128), [pk[2]], ["Vst"])

            def stage2(i):
                t, src, stile, qi = tiles[i]
                b = i % 2
                if i + 1 < len(tiles):
                    load_rope(i + 1)
                j = 1 if t >= 32 else 0
                y, ysq, qk, small = ybuf[b], tmpAB[b].rearrange("p (h d) -> p h d", d=128), qkbuf[b], smallb[b]
                tmp = [tmpA[b], tmpB[b]]
                KY, KQ, KS, KT0, KT1 = "y%d" % b, "qk%d" % b, "small%d" % b, "tmpA%d" % b, "tmpB%d" % b
                nh = 10 if qi is not None else 2
                h0 = 0 if qi is not None else 8
                yv = y[:, h0:10, :]
                if CUT < 3:
                    return
                ACTV(ysq[:, h0:10, :], yv, AF.Square, [KY], [KT0, KT1])
                RSUM('dve', small[:, 8 + h0:18], ysq[:, h0:10, :], [KT0, KT1], [KS])
                yield
                ACTV(small[:, 8 + h0:18], small[:, 8 + h0:18], AF.Sqrt, [KS, "epsb"], [KS], scale=1.0 / 128, bias=epsb)
                yield
                S.op('dve', lambda e, h0=h0, small=small: e.reciprocal(small[:, 8 + h0:18], small[:, 8 + h0:18]), [KS], [KS])
                TT('dve', yv, yv, small[:, 8 + h0:18].unsqueeze(2).to_broadcast([128, nh, 128]), ALU.mult, [KY, KS], [KY])
                TT('dve', yv, yv, qkgb[:, h0:10, :], ALU.mult, [KY, "qkgb"], [KY])
                if CUT < 4:
                    return
                y4 = y.rearrange("p h (a t f) -> p h a t f", a=2, t=2)
                q4 = qk.rearrange("p h (a t f) -> p h a t f", a=2, t=2)
                x1 = y4[:, h0:10, :, 0, :]
                x2 = y4[:, h0:10, :, 1, :]
                cb_ = cs[b][0].unsqueeze(1).to_broadcast([128, nh, 2, 32])
                sb_ = cs[b][1].unsqueeze(1).to_broadcast([128, nh, 2, 32])
                t0 = tmp[0][:, h0:10]
                t1 = tmp[1][:, h0:10]
                ck = "cs%d" % b
                TT('dve', t0, x1, cb_, ALU.mult, [KY, ck], [KT0])
                TT('dve', t1, x2, sb_, ALU.mult, [KY, ck], [KT1])
                TT('dve', q4[:, h0:10, :, 0, :], t0, t1, ALU.subtract, [KT0, KT1], [KQ])
                TT('dve', t0, x1, sb_, ALU.mult, [KY, ck], [KT0])
                TT('dve', t1, x2, cb_, ALU.mult, [KY, ck], [KT1])
                TT('dve', q4[:, h0:10, :, 1, :], t0, t1, ALU.add, [KT0, KT1], [KQ])
                yield
                if CUT < 5:
                    return
                if qi is not None:
                    pv = psbf(4).rearrange("p (h n) -> p h n", n=128)
                    for h in range(8):
                        TR(pv[:, h, :], qk[:, h, :], ident, [KQ, "ident"], [pk[4]])
                    CP('act', qT[:, :, qi, :], pv, [pk[4]], ["qT"])
                pv2 = psbf(5).rearrange("p (h n) -> p h n", n=128)
                for g in range(2):
                    TR(pv2[:, g, :], qk[:, 8 + g, :], ident, [KQ, "ident"], [pk[5]])
                yield
                CP('act', kT[:, :, t, :], pv2[:, 0:2, :], [pk[5]], ["kT"])


            def rr(gens):
                gens = [g_ for g_ in gens if g_ is not None]
                while gens:
                    for g_ in list(gens):
                        try:
                            next(g_)
                        except StopIteration:
                            gens.remove(g_)

            rr([stage1(0)])
            if len(tiles) > 1:
                rr([stage1(1), stage1b(0)])
            else:
                rr([stage1b(0)])
            for i in range(len(tiles)):
                rr([stage2(i),
                    stage1b(i + 1) if i + 1 < len(tiles) else None,
                    stage1(i + 2) if i + 2 < len(tiles) else None])

            if l == 0:
                qtiles = list(range(NT_ST))
            else:
                qtiles = list(range(16))

            def load_res(i):
                si = qtiles[i]
                b = i % 2
                if l == 0:
                    stile = si if si <= 16 else si + 15
                    src = xin
                else:
                    stile, src = si, xs
                DMA('sp', xt[b], src[stile * 128:(stile + 1) * 128, :], "xl%d" % b, [], ["xt%d" % b])

            if SMALL:
                qtiles = [qq_ for qq_ in qtiles if qq_ in (0, 15, 17)][:SMALL]
            if CUT < 6:
                qtiles = []
            if qtiles:
                load_res(0)
            pti = 0
            for i, si in enumerate(qtiles):
                b = i % 2
                if i + 1 < len(qtiles):
                    load_res(i + 1)
                isctx = si >= 17
                if l == 0:
                    keys = [(32, None), (33, None)] if isctx else [(t, None) for t in range(NT_IN)]
                else:
                    prev = (si - 1, 0) if si > 0 else (16, 2)
                    nxt = (si + 1, 1) if si < 15 else (16, 3)
                    keys = [prev, (si, None), nxt, (32, None), (33, None)]
                if SMALL:
                    keys = [kk_ for kk_ in keys if kk_[0] in keep]
                for g in range(2):
                    nk = len(keys)
                    base = pti
                    pti += nk

                    def emit_st(ki, g=g, base=base, si=si, keys=keys):
                        t = keys[ki][0]
                        sb_i = 4 + ((base + ki) % 2)
                        MM(banks[sb_i][:].rearrange("p (r q) -> p r q", q=128), kT[:, g, t, :], qT[:, 4 * g:4 * g + 4, si, :], True, True,
                           ["kT", "qT"], [pk[sb_i]])

                    emit_st(0)
                    for ki, (t, mi) in enumerate(keys):
                        sb_i = 4 + ((base + ki) % 2)
                        pt = PT[(base + ki) % 2]
                        ptk = "PT%d" % ((base + ki) % 2)
                        if ki + 1 < nk:
                            emit_st(ki + 1)
                        ACTV(pt, banks[sb_i][:], AF.Exp, [pk[sb_i]], [ptk], scale=SCALE)
                        if mi is not None:
                            TT('dve', pt, pt, masks[:, mi, :], ALU.mult, [ptk, "masks"], [ptk])
                        if CUT < 7:
                            continue
                        for r in range(4):
                            MM(banks[r][:, 0:129], pt[:, r * 128:(r + 1) * 128], Vst[:, t, g, 0:129], ki == 0, ki == nk - 1,
                               [ptk, "Vst"], [pk[r]])
                    if CUT < 8:
                        continue
                    for r in range(4):
                        h = 4 * g + r
                        if l == 1:
                            TT('dve', den[:, h:h + 1], banks[r][:, 128:129], esink[:, h:h + 1], ALU.add, [pk[r], "esink"], ["den"])
                        else:
                            CP('dve', den[:, h:h + 1], banks[r][:, 128:129], [pk[r]], ["den"])
                        S.op('dve', lambda e, h=h: e.reciprocal(den[:, h:h + 1], den[:, h:h + 1]), ["den"], ["den"])
                        ACTV(ob[:, h, :], banks[r][:, 0:128], AF.Identity, [pk[r], "den"], ["ob"], scale=den[:, h:h + 1])
                if CUT < 9:
                    continue
                pv = psbf(6).rearrange("p (h n) -> p h n", n=128)
                for h in range(8):
                    TR(pv[:, h, :], ob[:, h, :], ident, ["ob", "ident"], [pk[6]])
                CP('dve', oT, pv, [pk[6]], ["oT"])
                j = 1 if isctx else 0
                for dh in range(2):
                    for h in range(8):
                        MM(banks[7][:], oT[:, h, :], wo[:, h, dh * 512:(dh + 1) * 512], h == 0, h == 7, ["oT", "wo"], [pk[7]])
                    TT('dve', rtmp[:, dh * 512:(dh + 1) * 512], banks[7][:], gates[0][j][:, dh * 512:(dh + 1) * 512], ALU.mult,
                       [pk[7], "gate0%d" % j], ["rtmp"])
                TT('pool', rtmp, rtmp, xt[b], ALU.add, ["rtmp", "xt%d" % b], ["rtmp"])
                DMA('sp', xs[si * 128:(si + 1) * 128, :], rtmp, "xst", ["rtmp"], [])
            S.barrier()

        def peer_phase(l):
            st['off'] = PBASE
            ntl = NT_ST if l == 0 else 16
            wq = alloc([128, 8, 2048], BF16)
            ks = alloc([128, 2, 128], BF16)
            GT = alloc([128, 128, 256], BF16)
            ub = [alloc([128, 2, 8, 128], BF16) for i in range(3)]
            vb = [alloc([128, 2, D], BF16) for i in range(3)]
            big = alloc([128, 2048], F32)
            wk = alloc([128, 256], F32)
            vtop = alloc([128, 16, 16], F32)
            ix = alloc([128, 16, 16], U32)
            ixf = alloc([128, 16, 16], F32)
            ts = alloc([128, 8, 16], F32)
            pos = alloc([128, 8, 16], U32)
            pa = alloc([128, 8, 16], U32)
            pbq = alloc([128, 8, 16], U32)
            af = alloc([128, 8, 16], F32)
            bf = alloc([128, 8, 16], F32)
            e1f = alloc([128, 8, 16], F32)
            e2f = alloc([128, 8, 16], F32)
            gf = alloc([128, 8, 16], F32)
            zz = alloc([128, 16], F32)
            eT = alloc([128, 3, 256], BF16)
            Aoh2 = [alloc([128, 8, 128], BF16) for i in range(2)]
            Boh2 = [alloc([128, 8, 128], BF16) for i in range(2)]
            h2T = [alloc([128, 8, 256], BF16) for i in range(2)]
            qpT = alloc([128, 16, 256], BF16)
            xt = alloc([128, D], F32)
            xres = alloc([128, D], F32)
            xn = alloc([128, D], BF16)
            small = alloc([128, 32], F32)
            xsb = [alloc([128, 256], F32) for i in range(3)]
            g1_ = [alloc([128, 256], F32) for i in range(3)]
            g2_ = [alloc([128, 256], BF16) for i in range(3)]
            WT = [alloc([128, 256], BF16) for i in range(3)]
            xg_ = [alloc([128, 256], BF16) for i in range(3)]
            rtmp = alloc([128, D], F32)
            if l == 0:
                print("peer arena bytes", st['off'] * 2, "of", ARENA * 2)

            DMA('pool', wq, w_q[l].rearrange("(kc p) f -> p kc f", p=128), "wt", [], ["wq"])
            DMA('pool', ks, ksT[l], "wt2", [], ["ks"])

            passes = []
            t_ = 0
            if ntl % 2 == 1:
                passes.append([ntl - 1])
            while t_ + 1 < ntl:
                passes.append([t_, t_ + 1])
                t_ += 2
            PS = int(os.environ.get("K_PSMALL", "0"))
            if PS:
                passes = passes[:PS]

            def sel_front(pi):
                ptiles = passes[pi]
                nt = len(ptiles)
                N = nt * 128
                hb = pi % 2
                hk = "h2T%d" % hb
                for k, si in enumerate(ptiles):
                    DMA('sp', xt, xs[si * 128:(si + 1) * 128, :], "xl0", [], ["xt"])
                    j = 1 if si >= 17 else 0
                    norm_mod(xt, "xt", 1, j, h2T[hb][:, :, k * 128:(k + 1) * 128], hk, rtmp, xn, small, 7, sqkey="rtmp")
                    yield
                for ch in range(16):
                    o_ = banks[7][:, (ch % 2) * 256:(ch % 2) * 256 + N]
                    for kc in range(8):
                        MM(o_, wq[:, kc, ch * 128:(ch + 1) * 128], h2T[hb][:, kc, 0:N], kc == 0, kc == 7, ["wq", hk], [pk[7]])
                    CP('act', qpT[:, ch, 0:N], o_, [pk[7]], ["qpT"])
                    yield
                for k, si in enumerate(ptiles):
                    for q_ in range(4):
                        for g4 in range(4):
                            gi = q_ * 4 + g4
                            MM(banks[7][:, g4 * 128:(g4 + 1) * 128], qpT[:, gi, k * 128:(k + 1) * 128], ks[:, gi % 2, :],
                               True, True, ["qpT", "ks"], [pk[7]])
                        ACTV(big[:, q_ * 512:(q_ + 1) * 512], banks[7][:], AF.Identity, [pk[7]], ["big"])
                        yield
                    sv = big.rearrange("p (g k) -> p g k", k=128)
                    for gi in range(16):
                        S.op('dve', lambda e, gi=gi: e.max(out=vtop[:, gi, 0:8], in_=sv[:, gi, :]), ["big"], ["vtop"])
                        S.op('dve', lambda e, gi=gi: e.max_index(out=ix[:, gi, 0:8], in_max=vtop[:, gi, 0:8], in_values=sv[:, gi, :]),
                             ["big", "vtop"], ["ix"])
                        S.op('dve', lambda e, gi=gi: e.match_replace(out=wk[:, 0:128], in_to_replace=vtop[:, gi, 0:8],
                                                                      in_values=sv[:, gi, :], imm_value=-1e30),
                             ["big", "vtop"], ["wk"])
                        S.op('dve', lambda e, gi=gi: e.max(out=vtop[:, gi, 8:16], in_=wk[:, 0:128]), ["wk"], ["vtop"])
                        S.op('dve', lambda e, gi=gi: e.max_index(out=ix[:, gi, 8:16], in_max=vtop[:, gi, 8:16], in_values=wk[:, 0:128]),
                             ["wk", "vtop"], ["ix"])
                        yield
                    CP('dve', ixf, ix, ["ix"], ["ixf"])
                    v4 = vtop.rearrange("p (h t) k -> p h t k", t=2)
                    i4 = ixf.rearrange("p (h t) k -> p h t k", t=2)
                    cand = big.rearrange("p (h a b) -> p h a b", a=16, b=16)
                    TT('dve', cand, v4[:, :, 0, :].unsqueeze(3).to_broadcast([128, 8, 16, 16]),
                       v4[:, :, 1, :].unsqueeze(2).to_broadcast([128, 8, 16, 16]), ALU.add, ["vtop"], ["big"])
                    yield
                    c3 = big.rearrange("p (h c) -> p h c", c=256)
                    for h in range(8):
                        S.op('dve', lambda e, h=h: e.max(out=ts[:, h, 0:8], in_=c3[:, h, :]), ["big"], ["ts"])
                        S.op('dve', lambda e, h=h: e.max_index(out=pos[:, h, 0:8], in_max=ts[:, h, 0:8], in_values=c3[:, h, :]),
                             ["big", "ts"], ["pos"])
                        S.op('dve', lambda e, h=h: e.match_replace(out=wk, in_to_replace=ts[:, h, 0:8], in_values=c3[:, h, :],
                                                                    imm_value=-1e30), ["big", "ts"], ["wk"])
                        S.op('dve', lambda e, h=h: e.max(out=ts[:, h, 8:16], in_=wk), ["wk"], ["ts"])
                        S.op('dve', lambda e, h=h: e.max_index(out=pos[:, h, 8:16], in_max=ts[:, h, 8:16], in_values=wk),
                             ["wk", "ts"], ["pos"])
                        yield
                    TS('dve', pa, pos, 4, None, ALU.logical_shift_right, None, ["pos"], ["pa"])
                    TS('dve', pbq, pos, 15, None, ALU.bitwise_and, None, ["pos"], ["pbq"])
                    CP('dve', af, pa, ["pa"], ["af"])
                    CP('dve', bf, pbq, ["pbq"], ["bf"])
                    yield
                    oh = big.rearrange("p (h k a) -> p h k a", k=16, a=16)
                    io4 = iota16.rearrange("p (x y a) -> p x y a", x=1, y=1).to_broadcast([128, 8, 16, 16])
                    for (sel, idxv, dst) in [(af, i4[:, :, 0, :], e1f), (bf, i4[:, :, 1, :], e2f)]:
                        TT('dve', oh, sel.unsqueeze(3).to_broadcast([128, 8, 16, 16]), io4, ALU.is_equal,
                           ["af", "bf", "iota16"], ["big"])
                        TT('dve', oh, oh, idxv.unsqueeze(2).to_broadcast([128, 8, 16, 16]), ALU.mult, ["big", "ixf"], ["big"])
                        RSUM('dve', dst, oh, ["big"], ["e12"])
                        yield
                    TT('dve', gf, ts, ts[:, :, 0:1].to_broadcast([128, 8, 16]), ALU.subtract, ["ts"], ["gf"])
                    ACTV(gf, gf, AF.Exp, ["gf"], ["gf"])
                    RSUM('dve', zz[:, 0:8], gf, ["gf"], ["zz"])
                    S.op('dve', lambda e: e.reciprocal(zz[:, 8:16], zz[:, 0:8]), ["zz"], ["zz"])
                    TT('dve', gf, gf, zz[:, 8:16].unsqueeze(2).to_broadcast([128, 8, 16]), ALU.mult, ["gf", "zz"], ["gf"])
                    yield
                    for q_, srcv in enumerate([e1f, e2f, gf]):
                        TR(banks[7][:, q_ * 128:(q_ + 1) * 128], srcv.rearrange("p h k -> p (h k)"), identf,
                           ["e12", "gf", "identf"], [pk[7]])
                    CP('act', eT[:, :, k * 128:(k + 1) * 128], banks[7][:, 0:384].rearrange("p (q n) -> p q n", n=128), [pk[7]], ["eT"])
                    yield

            def sel_back(pi):
                ptiles = passes[pi]
                gidx = 0
                for k, si in enumerate(ptiles):
                    for grp in range(16):
                        n0 = k * 128 + grp * 8
                        w = gidx % 2
                        gidx += 1
                        Aoh, Boh = Aoh2[w], Boh2[w]
                        ka, kb = "Aoh%d" % w, "Boh%d" % w
                        ic = iota_cb.unsqueeze(1).to_broadcast([128, 8, 128])
                        TT('dve', Aoh, ic, eT[:, 0, n0:n0 + 8].unsqueeze(2).to_broadcast([128, 8, 128]), ALU.is_equal,
                           ["iota_cb", "eT"], [ka])
                        TT('dve', Boh, ic, eT[:, 1, n0:n0 + 8].unsqueeze(2).to_broadcast([128, 8, 128]), ALU.is_equal,
                           ["iota_cb", "eT"], [kb])
                        TT('dve', Boh, Boh, eT[:, 2, n0:n0 + 8].unsqueeze(2).to_broadcast([128, 8, 128]), ALU.mult,
                           [kb, "eT"], [kb])
                        for q2 in range(2):
                            b_ = 4 + 2 * w + q2
                            for tkn in range(4):
                                nn = q2 * 4 + tkn
                                MM(banks[b_][:, tkn * 128:(tkn + 1) * 128], Aoh[:, nn, :], Boh[:, nn, :], True, True,
                                   [ka, kb], [pk[b_]])
                            CP('act', GT[:, :, n0 + q2 * 4:n0 + q2 * 4 + 4],
                               banks[b_][:].rearrange("p (n j) -> p j n", j=128), [pk[b_]], ["GT"])

            def chunk_loop(pi, gen):
                ptiles = passes[pi]
                nt = len(ptiles)
                N = nt * 128
                hb = pi % 2
                hk = "h2T%d" % hb
                NG = 64
                NTB = len(ub)
                DEP = len(xsb)

                def load_tab(gi_):
                    b = gi_ % NTB
                    DMA('pool', ub[b].rearrange("p j k c -> p j (k c)"),
                        uT[l, 2 * gi_:2 * gi_ + 2].rearrange("j p k c -> p j (k c)"), "tu%d" % b, [], ["ub%d" % b])
                    DMA('pool', vb[b], vT[l, 2 * gi_:2 * gi_ + 2].rearrange("j c d -> c j d"), "tv%d" % b, [], ["vb%d" % b])

                def u_mm(jc):
                    b = (jc // 2) % NTB
                    jj = jc % 2
                    sbk = 4 + jc % DEP
                    for kc in range(8):
                        MM(banks[sbk][:, 0:N], ub[b][:, jj, kc, :], h2T[hb][:, kc, 0:N], kc == 0, kc == 7,
                           ["ub%d" % b, hk], [pk[sbk]])

                for gi_ in range(min(NTB - 1, NG)):
                    load_tab(gi_)
                u_mm(0)
                u_mm(1)

                def front(jc):
                    w_ = jc % DEP
                    sbk = 4 + w_
                    xk, g1k, xgk = "xsb%d" % w_, "g1_%d" % w_, "xg%d" % w_
                    ACTV(xsb[w_][:, 0:N], banks[sbk][:, 0:N], AF.Identity, [pk[sbk]], [xk])
                    ACTV(g1_[w_][:, 0:N], banks[sbk][:, 0:N], AF.Square, [pk[sbk]], [g1k], scale=0.21145921592128275)
                    TT('pool', xg_[w_][:, 0:N], xsb[w_][:, 0:N], GT[:, jc, 0:N], ALU.mult, [xk, "GT"], [xgk])
                    STT('dve', g1_[w_][:, 0:N], g1_[w_][:, 0:N], 1.0, xsb[w_][:, 0:N], ALU.add, ALU.mult, [g1k, xk], [g1k])

                front(0)
                for jc in range(128):
                    gi_ = jc // 2
                    jj = jc % 2
                    b = gi_ % NTB
                    w_ = jc % DEP
                    if jj == 0 and gi_ + NTB - 1 < NG:
                        load_tab(gi_ + NTB - 1)
                    if jc + 2 < 128:
                        u_mm(jc + 2)
                    if jc + 1 < 128:
                        front(jc + 1)
                    g1k, g2k, wtk, xgk = "g1_%d" % w_, "g2_%d" % w_, "WT%d" % w_, "xg%d" % w_
                    ACTV(g2_[w_][:, 0:N], g1_[w_][:, 0:N], AF.Sigmoid, [g1k], [g2k], scale=1.5957691216057308)
                    TT('dve', WT[w_][:, 0:N], xg_[w_][:, 0:N], g2_[w_][:, 0:N], ALU.mult, [xgk, g2k], [wtk])
                    for k in range(nt):
                        for dh in range(2):
                            ab = k * 2 + dh
                            MM(banks[ab][:], WT[w_][:, k * 128:(k + 1) * 128], vb[b][:, jj, dh * 512:(dh + 1) * 512],
                               jc == 0, jc == 127, [wtk, "vb%d" % b], [pk[ab]])
                    if gen is not None and jc >= 2 and jc % 3 != 0:
                        next(gen, None)

            def residual(pi):
                ptiles = passes[pi]
                for k, si in enumerate(ptiles):
                    j = 1 if si >= 17 else 0
                    DMA('sp', xres, xs[si * 128:(si + 1) * 128, :], "xl1", [], ["xres"])
                    for dh in range(2):
                        ab = k * 2 + dh
                        TT('dve', rtmp[:, dh * 512:(dh + 1) * 512], banks[ab][:], gates[1][j][:, dh * 512:(dh + 1) * 512], ALU.mult,
                           [pk[ab], "gate1%d" % j], ["rtmp"])
                    TT('pool', rtmp, rtmp, xres, ALU.add, ["rtmp", "xres"], ["rtmp"])
                    dst = xs if l == 0 else out
                    DMA('sp', dst[si * 128:(si + 1) * 128, :], rtmp, "xst", ["rtmp"], [])

            def drain(gen):
                if gen is not None:
                    for _ in gen:
                        pass

            drain(sel_front(0))
            for pi in range(len(passes)):
                sel_back(pi)
                gen = sel_front(pi + 1) if pi + 1 < len(passes) else None
                chunk_loop(pi, gen)
                drain(gen)
                residual(pi)
            S.barrier()

        stop = int(os.environ.get("K_STOP", "99"))
        stage = 0
        for l in range(2):
            for ph in (mod_phase, attn_phase, peer_phase):
                if stage < stop:
                    ph(l)
                stage += 1
        if dbg is not None:
            if dbg[0] == "xs":
                DMA('sp', dbg_out, xs[dbg[1] * 128:(dbg[1] + 1) * 128, :], "misc", [], [])
            else:
                DMA('sp', dbg_out, arena[:, dbg[1] // 2:dbg[1] // 2 + 2048].bitcast(F32), "misc", [], [])
        S.barrier()
        S.emit()
    return nc


def rope_tables(pos):
    row = (pos // 64).astype(np.float32)
    col = (pos % 64).astype(np.float32)
    half = 64
    inv = (np.float32(10000.0) ** (-np.arange(0, half, 2, dtype=np.float32) / np.float32(half))).astype(np.float32)
    ang = np.concatenate([row[:, None] * inv, col[:, None] * inv], axis=-1).astype(np.float32)
    return np.cos(ang).astype(np.float32), np.sin(ang).astype(np.float32)


def make_in_maps(x, c, ctx, c_ctx, ada_w, ada_b, norm1_gain, norm2_gain, w_qkv, q_norm_gain,
                 k_norm_gain, w_o, attn_sinks, peer_w_q, peer_sub_keys, peer_u, peer_v):
    f = lambda a: np.ascontiguousarray(np.asarray(a, dtype=np.float32))
    x, c, ctx, c_ctx = f(x), f(c), f(ctx), f(c_ctx)
    shared = {}
    shared["ada_w"] = f(ada_w)
    ab = f(ada_b)
    shared["ada_b"] = ab
    shared["adabT"] = f(ab.reshape(2, 48, 128).transpose(0, 2, 1))
    shared["n1T"] = f(f(norm1_gain).reshape(2, 8, 128).transpose(0, 2, 1))
    shared["n2T"] = f(f(norm2_gain).reshape(2, 8, 128).transpose(0, 2, 1))
    shared["qkg"] = f(np.concatenate([np.tile(f(q_norm_gain), (1, 8)), np.tile(f(k_norm_gain), (1, 2))], axis=1))
    shared["w_qkv"] = f(w_qkv)
    shared["w_o"] = f(w_o)
    shared["peer_w_q"] = f(peer_w_q)
    shared["ksT"] = f(f(peer_sub_keys).transpose(0, 3, 1, 2))
    shared["uT"] = f(f(peer_u).reshape(2, 128, 128, 8, 128).transpose(0, 2, 4, 3, 1))
    shared["vT"] = f(f(peer_v).reshape(2, 128, 128, D).transpose(0, 2, 1, 3))
    shared["sinks"] = f(attn_sinks).reshape(1, 8)
    kk = np.arange(128)[:, None]
    qq = np.arange(128)[None, :]
    mprev = (qq <= kk).astype(np.float32)
    mnext = (kk <= qq).astype(np.float32)
    zero = np.zeros((128, 128), np.float32)
    in_maps = []
    for cid in range(8):
        b, half = cid // 2, cid % 2
        if half == 0:
            order = np.concatenate([np.arange(0, 2048), np.arange(2048, 2176), np.arange(2176, 4096)])
            mP0, mN15 = zero, mnext
        else:
            order = np.concatenate([np.arange(2048, 4096), np.arange(1920, 2048), np.arange(0, 1920)])
            mP0, mN15 = mprev, zero
        xin = np.concatenate([x[b][order], ctx[b]], axis=0)
        cos, sin = rope_tables(order)
        cosd = np.concatenate([cos, np.ones((256, 64), np.float32)], axis=0)
        sind = np.concatenate([sin, np.zeros((256, 64), np.float32)], axis=0)
        m = np.stack([np.tile(mm_, (1, 4)) for mm_ in (mprev, mnext, mP0, mN15)], axis=1)
        cT = np.stack([c[b].reshape(8, 128).T, c_ctx.reshape(8, 128).T], axis=2)
        d = dict(shared)
        d.update(xin=f(xin), cosd=f(cosd), sind=f(sind), maskd=f(m), cT=f(cT))
        in_maps.append(d)
    return in_maps


_NC = {}


def kernel(**inputs):
    in_maps = make_in_maps(**inputs)
    if "nc" not in _NC:
        _NC["nc"] = build()
    res = run_bass_kernel_spmd(_NC["nc"], in_maps, core_ids=list(range(8)))
    outp = np.zeros((4, 4096, D), np.float32)
    for cid in range(8):
        b, half = cid // 2, cid % 2
        outp[b, half * 2048:(half + 1) * 2048] = np.asarray(res.results[cid]["out"], dtype=np.float32)
    return outp
```
